# Optimizing a Trainium2 kernel written in Bass

```python
import math
import jax, jax.numpy as jnp
from jax import lax
import numpy as np

D_MODEL = 1024
BATCH = 2
SEQ = 16384
DEPTH = 4

N_MIXERS = 4
EPS = 1e-6
D_FF = 2816
REL_BUCKETS = 32
REL_MAX_DIST = 2048
REL_HEADS = 8
A_HEADS = 8
A_HEAD_DIM = D_MODEL // A_HEADS
IDX_HEADS = 8
IDX_DIM = 64
TOPK_MAX = 256
Q_BLOCK = 128
A_SPLITS = (D_MODEL, 2 * D_MODEL, 3 * D_MODEL,
            3 * D_MODEL + IDX_HEADS * IDX_DIM,
            3 * D_MODEL + IDX_HEADS * IDX_DIM + IDX_DIM)
A_IN = 3 * D_MODEL + IDX_HEADS * IDX_DIM + IDX_DIM + IDX_HEADS
B_CHUNK = 128
B_HALF = 3 * D_MODEL
B_GROUPS = 8
C_KERNEL = 31
D_PAIRS = ((128, 1), (512, 4), (2048, 16))
D_NGROUPS = len(D_PAIRS)
D_HEADS = 8
D_HEAD_DIM = 64
D_BLOCK = 128
D_SPAN = D_BLOCK * max(dil for _, dil in D_PAIRS)
D_IN = 3 * D_NGROUPS * D_HEADS * D_HEAD_DIM
D_OUT = D_HEADS * D_HEAD_DIM


def n_uses(m):
    return (DEPTH - m + N_MIXERS - 1) // N_MIXERS


kernel_name = "hybrid_interleaved_dsa_gmlp_conv_dilated"


def rmsnorm(x, g):
    xf = x.astype(jnp.float32)
    y = xf * lax.rsqrt(jnp.mean(xf * xf, axis=-1, keepdims=True) + EPS)
    return (y * g.astype(jnp.float32)).astype(x.dtype)


def layernorm(x, g, b):
    xf = x.astype(jnp.float32)
    mu = jnp.mean(xf, axis=-1, keepdims=True)
    var = jnp.mean(jnp.square(xf - mu), axis=-1, keepdims=True)
    y = (xf - mu) * lax.rsqrt(var + EPS)
    return (y * g.astype(jnp.float32) + b.astype(jnp.float32)).astype(x.dtype)


def swiglu(x, w_in, w_out):
    gate, up = jnp.split(x @ w_in, 2, axis=-1)
    return (jax.nn.silu(gate) * up) @ w_out


def rel_bucket(dist):
    max_exact = REL_BUCKETS // 2
    d = jnp.maximum(dist, 0)
    df = jnp.maximum(d, 1).astype(jnp.float32)
    large = max_exact + (jnp.log(df / max_exact) / math.log(REL_MAX_DIST / max_exact)
                         * (REL_BUCKETS - max_exact)).astype(jnp.int32)
    large = jnp.minimum(large, REL_BUCKETS - 1)
    return jnp.where(d < max_exact, d, large)


def mixer_a(h, w_in, w_out, rel_table):
    B_, S, _ = h.shape
    q, k, v, qi, ki, wi = jnp.split(h @ w_in, A_SPLITS, axis=-1)
    q = q.reshape(B_, S, A_HEADS, A_HEAD_DIM)
    k = k.reshape(B_, S, A_HEADS, A_HEAD_DIM)
    v = v.reshape(B_, S, A_HEADS, A_HEAD_DIM)
    qi = qi.reshape(B_, S, IDX_HEADS, IDX_DIM)
    wi = wi * (IDX_HEADS ** -0.5)
    top_k = min(TOPK_MAX, S // 4)
    nb = S // Q_BLOCK
    key_pos = jnp.arange(S)
    scale = A_HEAD_DIM ** -0.5
    gather = jax.vmap(lambda arr, ix: arr[ix])

    def block(i):
        start = i * Q_BLOCK
        q_pos = start + jnp.arange(Q_BLOCK)
        qb = lax.dynamic_slice_in_dim(q, start, Q_BLOCK, axis=1)
        qib = lax.dynamic_slice_in_dim(qi, start, Q_BLOCK, axis=1)
        wib = lax.dynamic_slice_in_dim(wi, start, Q_BLOCK, axis=1)
        rel = jax.nn.relu(jnp.einsum('bthd,bsd->bths', qib, ki).astype(jnp.float32) * (IDX_DIM ** -0.5))
        score = jnp.einsum('bth,bths->bts', wib.astype(jnp.float32), rel)
        causal = key_pos[None, :] <= q_pos[:, None]
        score = jnp.where(causal[None], score, -jnp.inf)
        _, idx = lax.top_k(score, top_k)
        valid = idx <= q_pos[None, :, None]
        kg = gather(k, idx)
        vg = gather(v, idx)
        bias = rel_table[rel_bucket(q_pos[None, :, None] - idx)]
        logits = (jnp.einsum('bthe,btkhe->bhtk', qb, kg).astype(jnp.float32) * scale
                  + jnp.transpose(bias, (0, 3, 1, 2)).astype(jnp.float32))
        logits = jnp.where(valid[:, None], logits, -jnp.inf)
        p = jax.nn.softmax(logits, axis=-1)
        o = jnp.einsum('bhtk,btkhe->bthe', p, vg)
        return o.reshape(B_, Q_BLOCK, D_MODEL)

    out = lax.map(block, jnp.arange(nb))
    out = jnp.transpose(out, (1, 0, 2, 3)).reshape(B_, S, D_MODEL).astype(h.dtype)
    return out @ w_out


def mixer_b(h, w_in, b_in, ln_g, ln_b, w_sp, b_sp, w_out):
    B_, S, _ = h.shape
    z = jax.nn.gelu(h @ w_in + b_in)
    u, v = jnp.split(z, 2, axis=-1)
    v = layernorm(v, ln_g, ln_b)
    nc = S // B_CHUNK
    v = v.reshape(B_, nc, B_CHUNK, B_GROUPS, B_HALF // B_GROUPS)
    tri = jnp.tril(jnp.ones((B_CHUNK, B_CHUNK), dtype=bool))
    w = jnp.where(tri[None], w_sp, jnp.zeros((), w_sp.dtype))
    sv = jnp.einsum('gts,bcsgd->bctgd', w, v) + jnp.transpose(b_sp)[None, None, :, :, None]
    y = u * sv.reshape(B_, S, B_HALF)
    return y @ w_out


def mixer_c(h, w_pw1, b_pw1, w_dw, b_dw, ln_g, ln_b, w_pw2, b_pw2):
    a, g = jnp.split(h @ w_pw1 + b_pw1, 2, axis=-1)
    y = a * jax.nn.sigmoid(g)
    y = lax.conv_general_dilated(y, w_dw[:, None, :], window_strides=(1,),
                                 padding=((C_KERNEL - 1, 0),),
                                 dimension_numbers=('NWC', 'WIO', 'NWC'),
                                 feature_group_count=D_MODEL) + b_dw
    y = jax.nn.silu(layernorm(y, ln_g, ln_b))
    return y @ w_pw2 + b_pw2


def dilated_group(q, k, v, window, dil, rel_table):
    B_, Sp, H, E = q.shape
    n = Sp // dil
    nb = n // D_BLOCK

    def to_sub(t):
        t = jnp.swapaxes(t.reshape(B_, n, dil, H, E), 1, 2)
        return t.reshape(B_, dil, nb, D_BLOCK, H, E)

    def band(t):
        prev = jnp.pad(t, ((0, 0), (0, 0), (1, 0), (0, 0), (0, 0), (0, 0)))[:, :, :-1]
        return jnp.concatenate([prev, t], axis=3)

    qs = to_sub(q)
    kb = band(to_sub(k))
    vb = band(to_sub(v))
    steps = window // dil
    p_loc = jnp.arange(D_BLOCK)[:, None]
    j_loc = jnp.arange(2 * D_BLOCK)[None, :]
    m = p_loc + D_BLOCK - j_loc
    in_win = (m >= 0) & (m <= steps)
    first = (jnp.arange(nb) == 0)[:, None, None] & (j_loc < D_BLOCK)[None]
    mask = in_win[None] & ~first
    bias = jnp.transpose(rel_table[rel_bucket(m * dil)], (2, 0, 1)).astype(jnp.float32)
    logits = jnp.einsum('brnqhe,brnkhe->brnhqk', qs, kb).astype(jnp.float32) * (E ** -0.5) + bias
    logits = jnp.where(mask[None, None, :, None], logits, -jnp.inf)
    mx = jnp.max(logits, axis=-1, keepdims=True)
    e = jnp.exp(logits - mx)
    l = jnp.sum(e, axis=-1, keepdims=True)
    o = jnp.einsum('brnhqk,brnkhe->brnqhe', e / l, vb)
    lse = jnp.swapaxes((mx + jnp.log(l))[..., 0], -1, -2)

    def from_sub(t):
        rest = t.shape[4:]
        t = t.reshape((B_, dil, n) + rest)
        return jnp.swapaxes(t, 1, 2).reshape((B_, Sp) + rest)

    return from_sub(o), from_sub(lse)


def mixer_d(h, w_in, w_out, rel_table):
    B_, S, _ = h.shape
    Sp = -(-S // D_SPAN) * D_SPAN
    proj = jnp.pad(h @ w_in, ((0, 0), (0, Sp - S), (0, 0)))
    proj = proj.reshape(B_, Sp, 3, D_NGROUPS, D_HEADS, D_HEAD_DIM)
    outs, lses = [], []
    for g, (window, dil) in enumerate(D_PAIRS):
        o, lse = dilated_group(proj[:, :, 0, g], proj[:, :, 1, g], proj[:, :, 2, g],
                               window, dil, rel_table)
        outs.append(o)
        lses.append(lse)
    alpha = jax.nn.softmax(jnp.stack(lses, 0), axis=0)
    y = jnp.sum(alpha[..., None] * jnp.stack(outs, 0), axis=0)
    y = y[:, :S].reshape(B_, S, D_OUT).astype(h.dtype)
    return y @ w_out


def setup_inputs(seed: int = 0) -> dict:
    key = jax.random.key(seed)
    ks = jax.random.split(key, 32)
    nrm = lambda k, shape, s: jax.random.normal(k, shape, jnp.float32) * s
    nA, nB, nC, nD = n_uses(0), n_uses(1), n_uses(2), n_uses(3)
    D = D_MODEL
    return {
        "x": nrm(ks[0], (BATCH, SEQ, D), 1.0),
        "norm_g": 1.0 + nrm(ks[1], (DEPTH, 3, D), 0.05),
        "final_g": 1.0 + nrm(ks[2], (D,), 0.05),
        "ffn_w_in": nrm(ks[3], (DEPTH, 2, D, 2 * D_FF), D ** -0.5),
        "ffn_w_out": nrm(ks[4], (DEPTH, 2, D_FF, D), D_FF ** -0.5),
        "rel_table": nrm(ks[5], (REL_BUCKETS, REL_HEADS), 0.5),
        "a_w_in": nrm(ks[6], (nA, D, A_IN), D ** -0.5),
        "a_w_out": nrm(ks[7], (nA, D, D), D ** -0.5),
        "b_w_in": nrm(ks[8], (nB, D, 2 * B_HALF), D ** -0.5),
        "b_b_in": nrm(ks[9], (nB, 2 * B_HALF), 0.02),
        "b_ln_g": 1.0 + nrm(ks[10], (nB, B_HALF), 0.05),
        "b_ln_b": nrm(ks[11], (nB, B_HALF), 0.02),
        "b_w_sp": nrm(ks[12], (nB, B_GROUPS, B_CHUNK, B_CHUNK), B_CHUNK ** -0.5),
        "b_b_sp": 1.0 + nrm(ks[13], (nB, B_GROUPS, B_CHUNK), 0.05),
        "b_w_out": nrm(ks[14], (nB, B_HALF, D), B_HALF ** -0.5),
        "c_w_pw1": nrm(ks[15], (nC, D, 2 * D), D ** -0.5),
        "c_b_pw1": nrm(ks[16], (nC, 2 * D), 0.02),
        "c_w_dw": nrm(ks[17], (nC, C_KERNEL, D), C_KERNEL ** -0.5),
        "c_b_dw": nrm(ks[18], (nC, D), 0.02),
        "c_ln_g": 1.0 + nrm(ks[19], (nC, D), 0.05),
        "c_ln_b": nrm(ks[20], (nC, D), 0.02),
        "c_w_pw2": nrm(ks[21], (nC, D, D), D ** -0.5),
        "c_b_pw2": nrm(ks[22], (nC, D), 0.02),
        "d_w_in": nrm(ks[23], (nD, D, D_IN), D ** -0.5),
        "d_w_out": nrm(ks[24], (nD, D_OUT, D), D_OUT ** -0.5),
    }


def reference(x, norm_g, final_g, ffn_w_in, ffn_w_out, rel_table,
              a_w_in, a_w_out,
              b_w_in, b_b_in, b_ln_g, b_ln_b, b_w_sp, b_b_sp, b_w_out,
              c_w_pw1, c_b_pw1, c_w_dw, c_b_dw, c_ln_g, c_ln_b, c_w_pw2, c_b_pw2,
              d_w_in, d_w_out):
    h = x
    for i in range(DEPTH):
        kind, j = i % N_MIXERS, i // N_MIXERS
        h = h + 0.5 * swiglu(rmsnorm(h, norm_g[i, 0]), ffn_w_in[i, 0], ffn_w_out[i, 0])
        hn = rmsnorm(h, norm_g[i, 1])
        if kind == 0:
            y = mixer_a(hn, a_w_in[j], a_w_out[j], rel_table)
        elif kind == 1:
            y = mixer_b(hn, b_w_in[j], b_b_in[j], b_ln_g[j], b_ln_b[j], b_w_sp[j], b_b_sp[j], b_w_out[j])
        elif kind == 2:
            y = mixer_c(hn, c_w_pw1[j], c_b_pw1[j], c_w_dw[j], c_b_dw[j], c_ln_g[j], c_ln_b[j],
                        c_w_pw2[j], c_b_pw2[j])
        else:
            y = mixer_d(hn, d_w_in[j], d_w_out[j], rel_table)
        h = h + y
        h = h + 0.5 * swiglu(rmsnorm(h, norm_g[i, 2]), ffn_w_in[i, 1], ffn_w_out[i, 1])
    return rmsnorm(h, final_g)
```

```python
import numpy as np
from contextlib import ExitStack
import concourse.bass as bass
import concourse.mybir as mybir
from concourse.bass_utils import run_bass_kernel_spmd

F32 = mybir.dt.float32
BF16 = mybir.dt.bfloat16
ALU = mybir.AluOpType
AF = mybir.ActivationFunctionType
AX = mybir.AxisListType

D = 1024
KC = 8
DFF = 2816
NFF = 22
EPS = 1e-6
NCORES = 8
ENGS = ("pe", "act", "dve", "pool", "sp")
HT = [f"ht{k}" for k in range(8)]
XN = [f"xn{k}" for k in range(8)]


class Prog:
    def __init__(self, nc):
        self.nc = nc
        self.q = {e: [] for e in ENGS}
        self.seen = {e: {} for e in ENGS}
        self.lastw = {}
        self.readers = {}
        self.dmacnt = {}
        self.st = ExitStack()
        self.n_sb = 0

    def sb(self, shape, dt, name=None):
        self.n_sb += 1
        return self.st.enter_context(self.nc.sbuf_tensor(name or f"sb{self.n_sb}", list(shape), dt))

    def ps(self, shape, dt=F32, name=None):
        self.n_sb += 1
        return self.st.enter_context(self.nc.psum_tensor(name or f"ps{self.n_sb}", list(shape), dt))

    def _deps(self, eng, r, w):
        deps = {}

        def add(k, v):
            if deps.get(k, -1) < v:
                deps[k] = v

        for b in r:
            t = self.lastw.get(b)
            if t is not None:
                add(*t)
        for b in w:
            t = self.lastw.get(b)
            if t is not None:
                add(*t)
            for k, v in self.readers.get(b, {}).items():
                add(k, v)
        waits = []
        for k, v in deps.items():
            if k == eng and eng in ("pe", "sp"):
                continue
            if self.seen[eng].get(k, -1) >= v:
                continue
            self.seen[eng][k] = v
            waits.append((k, v))
        return waits

    def _note(self, tok, r, w):
        k, v = tok
        for b in r:
            d = self.readers.setdefault(b, {})
            if d.get(k, -1) < v:
                d[k] = v
        for b in w:
            self.lastw[b] = tok
            self.readers[b] = {}

    def op(self, eng, fn, r=(), w=()):
        waits = self._deps(eng, r, w)
        idx = len(self.q[eng])
        self.q[eng].append((waits, fn, None))
        self._note((eng, idx), r, w)

    def dma(self, queue, out, in_, sem, r=(), w=(), **kw):
        waits = self._deps(queue, r, w)
        key = "d:" + sem
        cum = self.dmacnt.get(key, 0) + 16
        self.dmacnt[key] = cum
        self.q[queue].append((waits, lambda e: e.dma_start(out=out, in_=in_, **kw), key))
        self._note((key, cum), r, w)

    def wait_bufs(self, eng, bufs):
        waits = self._deps(eng, bufs, ())
        self.q[eng].append((waits, None, None))

    def emit(self):
        nc = self.nc
        EP = 60000
        DEP = 3750
        needed = {e: set() for e in ENGS}
        for e in ENGS:
            for waits, fn, dk in self.q[e]:
                for k, v in waits:
                    if k in needed:
                        needed[k].add(v)
        val = {}
        nep = {}
        for e in ENGS:
            c = 0
            m = {}
            for i in range(len(self.q[e])):
                if i in needed[e]:
                    m[i] = (c // EP, c % EP + 1)
                    c += 1
            val[e] = m
            nep[e] = (c + EP - 1) // EP
        sems = {}
        nsem = 0
        for e in ENGS:
            for ep in range(nep[e]):
                sems[(e, ep)] = self.st.enter_context(nc.semaphore(f"s_{e}_{ep}"))
                nsem += 1
        for k in sorted(self.dmacnt.keys()):
            n = self.dmacnt[k] // 16
            for ep in range((n + DEP - 1) // DEP):
                sems[(k, ep)] = self.st.enter_context(nc.semaphore("s_" + k.replace(":", "_") + f"_{ep}"))
                nsem += 1
        block = self.st.enter_context(nc.Block())
        q = self.q
        dcount = {}

        def dsem(k, cum):
            n = cum // 16 - 1
            return sems[(k, n // DEP)], (n % DEP + 1) * 16

        def run(ename, eng):
            for i, (waits, fn, dk) in enumerate(q[ename]):
                for k, v in waits:
                    if k in val:
                        ep, vv = val[k][v]
                        eng.wait_ge(sems[(k, ep)], vv)
                    else:
                        sm, vv = dsem(k, v)
                        eng.wait_ge(sm, vv)
                if fn is None:
                    continue
                ins = fn(eng)
                if dk is not None:
                    dcount[dk] = dcount.get(dk, 0) + 16
                    sm, _ = dsem(dk, dcount[dk])
                    ins.then_inc(sm, 16)
                elif i in val[ename]:
                    ins.then_inc(sems[(ename, val[ename][i][0])], 1)

        block.tensor(lambda eng: run("pe", eng))
        block.scalar(lambda eng: run("act", eng))
        block.vector(lambda eng: run("dve", eng))
        block.gpsimd(lambda eng: run("pool", eng))
        block.sync(lambda eng: run("sp", eng))
        self.st.close()
        self.stats = {e: len(self.q[e]) for e in ENGS}
        self.nsem = nsem


def _barrier(self):
    toks = []
    for e in ENGS:
        for i in range(len(self.q[e]) - 1, -1, -1):
            w_, fn, dk = self.q[e][i]
            if fn is not None and dk is None:
                toks.append((e, i))
                break
    for k, cum in self.dmacnt.items():
        toks.append((k, cum))
    for e in ENGS:
        waits = []
        for k, v in toks:
            if self.seen[e].get(k, -1) >= v:
                continue
            self.seen[e][k] = v
            waits.append((k, v))
        self.q[e].append((waits, None, None))
    self.lastw.clear()
    self.readers.clear()


Prog.barrier = _barrier

RAW_BYTES = 206 * 1024


class Ctx:
    def __init__(self, nc, TT):
        self.nc = nc
        self.p = p = Prog(nc)
        self.TT = TT
        self.NS = TT // 512
        self.RAW = p.sb([128, RAW_BYTES // 2], BF16, "raw")
        self.PS = p.ps([128, 4096], F32, "psum")
        self.cnt = {}
        self.rowlocal_map()

    def view(self, off, shape, dt, p0=0):
        n = int(np.prod(shape[1:]))
        nb = n * (4 if dt == F32 else 2)
        assert off % 4 == 0 and off + nb <= RAW_BYTES, (off, shape)
        a = self.RAW[p0:p0 + shape[0], off // 2: off // 2 + nb // 2]
        if dt == F32:
            a = a.bitcast(F32)
        if len(shape) == 3:
            a = a.rearrange("p (a b) -> p a b", a=shape[1])
        elif len(shape) == 4:
            a = a.rearrange("p (a b c) -> p a b c", a=shape[1], b=shape[2])
        return a

    def bank(self, b):
        return self.PS[:, b * 512:(b + 1) * 512]

    def nxt(self, k, n=2):
        v = self.cnt.get(k, 0)
        self.cnt[k] = v + 1
        return v % n

    def rowlocal_map(self):
        TT = self.TT
        o = 0
        self.ht = self.view(o, [128, KC, TT], F32); o += KC * TT * 4
        self.xn = self.view(o, [128, KC, TT], BF16); o += KC * TT * 2
        self.sq = self.view(o, [128, KC, TT], BF16); self.o_sq = o; o += KC * TT * 2
        self.act = self.view(o, [128, 24, TT], BF16); self.o_act = o; o += 24 * TT * 2
        self.wgu = [self.view(o + i * 8192, [128, KC, 512], BF16) for i in range(2)]; o += 16384
        self.wo = [self.view(o + i * 6144, [128, 24, 128], BF16) for i in range(2)]; o += 12288
        self.tmp = [self.view(o + i * 2048, [128, 512], F32) for i in range(2)]; o += 4096
        self.rstd = [self.view(o + i * 2048, [128, 512], F32) for i in range(2)]; o += 4096
        self.ones = self.view(o, [128, 128], BF16); o += 256
        self.gcols = self.view(o, [128, 16, KC], F32); o += 512
        self.o_misc = o
        self.pm = [self.bank(i) for i in range(4)]
        self.po = [self.bank(4 + i) for i in range(2)]
        self.pst = [self.bank(6 + i) for i in range(2)]

    def init_consts(self):
        self.p.op("dve", lambda e: e.memset(self.ones, 1.0), w=["ones"])


ACTK = [f"act{j}" for j in range(24)]


def load_ht(c, src_ap, t0):
    c.p.dma("sp", c.ht, src_ap[:, :, t0:t0 + c.TT].rearrange("k p t -> p k t"), "ht", r=["dram_h"], w=HT)


def store_ht(c, dst_ap, t0, key="dram_h"):
    c.p.dma("sp", dst_ap[:, :, t0:t0 + c.TT].rearrange("k p t -> p k t"), c.ht, "hst", r=HT, w=[key])


def load_gcols(c, g_ap, n):
    c.p.dma("sp", c.gcols[:, 0:n, :], g_ap, "gcols", w=["gcols"])


def rmsnorm(c, gi):
    p = c.p
    ht, xn, sq = c.ht, c.xn, c.sq
    p.op("act", lambda e: e.activation(out=sq, in_=ht, func=AF.Square), r=HT, w=["sq"])
    for s in range(c.NS):
        sl = slice(s * 512, (s + 1) * 512)
        b = c.nxt("pst")
        pst = c.pst[b]
        for kc in range(KC):
            p.op("pe", lambda e, kc=kc, pst=pst, sl=sl: e.matmul(pst, c.ones, sq[:, kc, sl], start=(kc == 0), stop=(kc == KC - 1)),
                 r=["sq", "ones"], w=[f"pst{b}"])
        rb = c.nxt("rstd")
        rstd = c.rstd[rb]
        tb = c.nxt("tmp")
        tmp = c.tmp[tb]
        p.op("act", lambda e, pst=pst, tmp=tmp: e.activation(out=tmp, in_=pst, func=AF.Sqrt, bias=EPS, scale=1.0 / D),
             r=[f"pst{b}"], w=[f"tmp{tb}"])
        p.op("dve", lambda e, rstd=rstd, tmp=tmp: e.reciprocal(out=rstd, in_=tmp), r=[f"tmp{tb}"], w=[f"rstd{rb}"])
        for kc in range(KC):
            p.op("dve", lambda e, kc=kc, rstd=rstd, sl=sl: e.scalar_tensor_tensor(
                out=xn[:, kc, sl], in0=ht[:, kc, sl], scalar=c.gcols[:, gi, kc:kc + 1], in1=rstd, op0=ALU.mult, op1=ALU.mult),
                r=[f"ht{kc}", f"rstd{rb}", "gcols"], w=[f"xn{kc}"])


def glu_in(c, win_ap, nch, epi):
    p = c.p
    for j in range(nch):
        wb = c.nxt("wgu")
        wgu = c.wgu[wb]
        p.dma("pool", wgu[:, :, 0:256], win_ap[j], f"wgu{wb}", w=[f"wgu{wb}"])
        for s in range(c.NS):
            sl = slice(s * 512, (s + 1) * 512)
            b = c.nxt("pg")
            pg, pu = c.pm[2 * b], c.pm[2 * b + 1]
            for kc in range(KC):
                p.op("pe", lambda e, kc=kc, pg=pg, wgu=wgu, sl=sl: e.matmul(pg, wgu[:, kc, 0:128], c.xn[:, kc, sl], start=(kc == 0), stop=(kc == KC - 1)),
                     r=[f"xn{kc}", f"wgu{wb}"], w=[f"pm{2 * b}"])
            for kc in range(KC):
                p.op("pe", lambda e, kc=kc, pu=pu, wgu=wgu, sl=sl: e.matmul(pu, wgu[:, kc, 128:256], c.xn[:, kc, sl], start=(kc == 0), stop=(kc == KC - 1)),
                     r=[f"xn{kc}", f"wgu{wb}"], w=[f"pm{2 * b + 1}"])
            epi(j, s, sl, pg, pu, f"pm{2 * b}", f"pm{2 * b + 1}")


def linear_out(c, wout_ap, nk, rhs, rkey, epi, n_oc=KC):
    p = c.p
    for oc in range(n_oc):
        wb = c.nxt("wo")
        wo = c.wo[wb]
        p.dma("pool", wo[:, 0:nk, :], wout_ap[oc], f"wo{wb}", w=[f"wo{wb}"])
        for s in range(c.NS):
            sl = slice(s * 512, (s + 1) * 512)
            b = c.nxt("po")
            po = c.po[b]
            for k in range(nk):
                p.op("pe", lambda e, k=k, po=po, wo=wo, sl=sl: e.matmul(po, wo[:, k, :], rhs(k, sl), start=(k == 0), stop=(k == nk - 1)),
                     r=[rkey(k), f"wo{wb}"], w=[f"po{b}"])
            epi(oc, s, sl, po, f"po{b}")


def ffn(c, win_ap, wout_ap):
    p = c.p

    def epi1(j, s, sl, pg, pu, kg, ku):
        tb = c.nxt("tmp")
        tmp = c.tmp[tb]
        p.op("act", lambda e: e.activation(out=tmp, in_=pg, func=AF.Silu), r=[kg], w=[f"tmp{tb}"])
        p.op("dve", lambda e: e.tensor_tensor(out=c.act[:, j, sl], in0=pu, in1=tmp, op=ALU.mult),
             r=[ku, f"tmp{tb}"], w=[f"act{j}"])

    glu_in(c, win_ap, NFF, epi1)

    def epi2(oc, s, sl, po, key):
        p.op("dve", lambda e: e.scalar_tensor_tensor(out=c.ht[:, oc, sl], in0=po, scalar=0.5, in1=c.ht[:, oc, sl], op0=ALU.mult, op1=ALU.add),
             r=[key, f"ht{oc}"], w=[f"ht{oc}"])

    linear_out(c, wout_ap, NFF, lambda k, sl: c.act[:, k, sl], lambda k: f"act{k}", epi2)


def res_add_epi(c, bias_col=None):
    p = c.p

    def epi(oc, s, sl, po, key):
        if bias_col is None:
            p.op("dve", lambda e: e.tensor_tensor(out=c.ht[:, oc, sl], in0=po, in1=c.ht[:, oc, sl], op=ALU.add),
                 r=[key, f"ht{oc}"], w=[f"ht{oc}"])
        else:
            p.op("dve", lambda e: e.scalar_tensor_tensor(out=c.ht[:, oc, sl], in0=po, scalar=bias_col(oc), in1=c.ht[:, oc, sl], op0=ALU.add, op1=ALU.add),
                 r=[key, f"ht{oc}", "cvec"], w=[f"ht{oc}"])
    return epi


def lay_pair(w, half):
    n = half // 128
    a = w[:, :half].reshape(KC, 128, n, 128)
    b = w[:, half:].reshape(KC, 128, n, 128)
    ab = np.concatenate([a, b], axis=3)
    return np.ascontiguousarray(ab.transpose(2, 1, 0, 3))


def lay_out(w):
    nk = w.shape[0] // 128
    no = w.shape[1] // 128
    return np.ascontiguousarray(w.reshape(nk, 128, no, 128).transpose(2, 1, 0, 3))


def lay_cols(w, width):
    n = w.shape[1] // width
    return np.ascontiguousarray(w.reshape(KC, 128, n, width).transpose(2, 1, 0, 3))


def lay_vec(v):
    n = v.shape[0]
    m = v.shape[1] // 128
    return np.ascontiguousarray(v.reshape(n, m, 128).transpose(2, 0, 1))


def lay_xT(x):
    return np.ascontiguousarray(x.T.reshape(KC, 128, x.shape[0]))


def unlay_xT(xt):
    return np.ascontiguousarray(xt.reshape(-1, xt.shape[2]).T)


def evac(c, out, in_, rk, wk, scale=None):
    p = c.p
    if c.nxt("evac") == 0:
        if scale is None:
            p.op("act", lambda e: e.activation(out=out, in_=in_, func=AF.Copy), r=rk, w=wk)
        else:
            p.op("act", lambda e: e.activation(out=out, in_=in_, func=AF.Copy, scale=scale), r=rk, w=wk)
    else:
        if scale is None:
            p.op("dve", lambda e: e.tensor_copy(out=out, in_=in_), r=rk, w=wk)
        else:
            p.op("dve", lambda e: e.tensor_scalar(out=out, in0=in_, scalar1=scale, scalar2=None, op0=ALU.mult), r=rk, w=wk)


def aproj(c, W, O, t0):
    p = c.p
    TT = c.TT
    nb = TT // 128
    qst = c.act[:, 0:8, :]
    kst = c.act[:, 8:16, :]
    qist = c.view(c.o_act + 16 * TT * 2, [64, TT // 16, 128], BF16)
    vst = c.view(c.o_sq, [128, nb, 1024], BF16)
    kist = c.view(c.o_misc, [64, TT], BF16)
    wist = c.view(c.o_misc + 2 * TT, [8, TT], F32)
    for which, wap, st, k0 in (("q", W["wq"], qst, 0), ("k", W["wk"], kst, 8)):
        for g2 in range(4):
            wb = c.nxt("wgu")
            wgu = c.wgu[wb]
            p.dma("pool", wgu[:, :, 0:256], wap[g2], f"wgu{wb}", w=[f"wgu{wb}"])
            for g in range(2):
                ch = g2 * 2 + g
                for s in range(c.NS):
                    sl = slice(s * 512, (s + 1) * 512)
                    b = c.nxt("pm", 4)
                    ps = c.pm[b]
                    for kc in range(KC):
                        p.op("pe", lambda e, kc=kc, ps=ps, wgu=wgu, sl=sl, g=g: e.matmul(ps, wgu[:, kc, g * 128:(g + 1) * 128], c.xn[:, kc, sl], start=(kc == 0), stop=(kc == KC - 1)),
                             r=[f"xn{kc}", f"wgu{wb}"], w=[f"pm{b}"])
                    evac(c, st[:, ch, sl], ps, [f"pm{b}"], [f"act{k0 + ch}"])
    for cg in range(2):
        wb = c.nxt("wgu")
        wgu = c.wgu[wb]
        p.dma("pool", wgu, W["wv"][cg], f"wgu{wb}", w=[f"wgu{wb}"])
        for tb in range(nb):
            b = c.nxt("pm", 4)
            ps = c.pm[b]
            for kc in range(KC):
                p.op("pe", lambda e, kc=kc, ps=ps, wgu=wgu, tb=tb: e.matmul(ps, c.xn[:, kc, tb * 128:(tb + 1) * 128], wgu[:, kc, :], start=(kc == 0), stop=(kc == KC - 1)),
                     r=[f"xn{kc}", f"wgu{wb}"], w=[f"pm{b}"])
            evac(c, vst[:, tb, cg * 512:(cg + 1) * 512], ps, [f"pm{b}"], ["sq"])
    wb = c.nxt("wgu")
    wgu = c.wgu[wb]
    p.dma("pool", wgu, W["wqi"][0], f"wgu{wb}", w=[f"wgu{wb}"])
    for h in range(8):
        for s in range(c.NS):
            sl = slice(s * 512, (s + 1) * 512)
            b = c.nxt("pm", 4)
            ps = c.pm[b]
            for kc in range(KC):
                p.op("pe", lambda e, kc=kc, ps=ps, wgu=wgu, sl=sl, h=h: e.matmul(ps[0:64, :], wgu[:, kc, h * 64:(h + 1) * 64], c.xn[:, kc, sl], start=(kc == 0), stop=(kc == KC - 1)),
                     r=[f"xn{kc}", f"wgu{wb}"], w=[f"pm{b}"])
            evac(c, qist[:, s * 32:(s + 1) * 32, h * 16:(h + 1) * 16], ps[0:64, :].rearrange("p (j q) -> p j q", q=16), [f"pm{b}"], [f"act{16 + hh}" for hh in range(8)])
    wb = c.nxt("wgu")
    wgu = c.wgu[wb]
    p.dma("pool", wgu[:, :, 0:72], W["wkw"][0], f"wgu{wb}", w=[f"wgu{wb}"])
    for s in range(c.NS):
        sl = slice(s * 512, (s + 1) * 512)
        b = c.nxt("pm", 4)
        ps = c.pm[b]
        for kc in range(KC):
            p.op("pe", lambda e, kc=kc, ps=ps, wgu=wgu, sl=sl: e.matmul(ps[0:64, :], wgu[:, kc, 0:64], c.xn[:, kc, sl], start=(kc == 0), stop=(kc == KC - 1)),
                 r=[f"xn{kc}", f"wgu{wb}"], w=[f"pm{b}"])
        evac(c, kist[:, sl], ps[0:64, :], [f"pm{b}"], ["kist"])
        b = c.nxt("pm", 4)
        ps = c.pm[b]
        for kc in range(KC):
            p.op("pe", lambda e, kc=kc, ps=ps, wgu=wgu, sl=sl: e.matmul(ps[0:8, :], wgu[:, kc, 64:72], c.xn[:, kc, sl], start=(kc == 0), stop=(kc == KC - 1)),
                 r=[f"xn{kc}", f"wgu{wb}"], w=[f"pm{b}"])
        evac(c, wist[:, sl], ps[0:8, :], [f"pm{b}"], ["wist"], scale=8 ** -0.5)
    p.dma("sp", O["qT"][:, :, t0:t0 + TT].rearrange("h p t -> p h t"), qst, "st_q", r=ACTK[0:8], w=["o_q"])
    for b_ in range(nb):
        p.dma("sp", O["kTb"][t0 // 128 + b_], kst[:, :, b_ * 128:(b_ + 1) * 128], "st_k", r=ACTK[8:16], w=["o_k"])
    p.dma("sp", O["v"][t0:t0 + TT, :].rearrange("(b p) n -> p b n", p=128), vst, "st_v", r=["sq"], w=["o_v"])
    p.dma("sp", O["qiT"][:, t0 * 8:(t0 + TT) * 8], qist.rearrange("p a b -> p (a b)"), "st_qi", r=ACTK[16:24], w=["o_qi"])
    p.dma("sp", O["kiT"][:, t0:t0 + TT], kist, "st_ki", r=["kist"], w=["o_ki"])
    p.dma("sp", O["wiT"][:, t0:t0 + TT], wist, "st_wi", r=["wist"], w=["o_wi"])


def rel_thresholds():
    d = np.arange(0, 4096, dtype=np.int64)
    df = np.maximum(d, 1).astype(np.float32)
    large = 16 + (np.log(df / np.float32(16)) / np.float32(np.log(2048 / 16)) * np.float32(16)).astype(np.int32)
    large = np.minimum(large, 31)
    bucket = np.where(d < 16, d, large)
    return [int(np.argmax(bucket >= b)) for b in range(1, 32)]


def gen_bias(c, DT, Mt, F, relt, delta, ncols, eng="dve"):
    p = c.p
    thr = rel_thresholds()
    for h in range(8):
        p.op(eng, lambda e, h=h: e.tensor_scalar(out=F[:, h, 0:ncols], in0=DT[:, 0:ncols], scalar1=0.0, scalar2=relt[:, h:h + 1], op0=ALU.mult, op1=ALU.add),
             r=["DT", "relt"], w=[f"F{h}"])
    for b in range(1, 32):
        p.op(eng, lambda e, b=b: e.tensor_scalar(out=Mt[:, 0:ncols], in0=DT[:, 0:ncols], scalar1=float(thr[b - 1]), scalar2=None, op0=ALU.is_ge),
             r=["DT"], w=["Mt"])
        for h in range(8):
            p.op(eng, lambda e, b=b, h=h: e.scalar_tensor_tensor(out=F[:, h, 0:ncols], in0=Mt[:, 0:ncols], scalar=delta[:, b * 8 + h:b * 8 + h + 1],
                                                                in1=F[:, h, 0:ncols], op0=ALU.mult, op1=ALU.add),
                 r=["Mt", "delta", f"F{h}"], w=[f"F{h}"])


def load_relt(c, relt, delta, relt_ap):
    p = c.p
    p.dma("sp", relt, relt_ap, "relt", w=["relt"])
    p.op("dve", lambda e: e.memset(delta[:, 0:8], 0.0), w=["delta"])
    p.op("dve", lambda e: e.tensor_tensor(out=delta[:, 8:256], in0=relt[:, 8:256], in1=relt[:, 0:248], op=ALU.subtract), r=["relt", "delta"], w=["delta"])


A_NIT = 16
A_NEAR = 20
A_MASK = -30000.0
A_SCALE = 128 ** -0.5
CPB = 4


def a_attention(c, I, O, S, T):
    p = c.p
    TTA = 512
    nslots = T // 128
    o = 0
    scores = c.view(o, [128, S], F32); o += 4 * S
    mterm = c.view(o, [128, S], BF16); o += 2 * S
    btiles = c.view(o, [128, A_NEAR + 1, 8, 128], BF16)
    btflat = c.view(o, [128, A_NEAR + 1, 1024], BF16); o += (A_NEAR + 1) * 8 * 128 * 2
    qTt = c.view(o, [128, 8, TTA], BF16); o += 8 * TTA * 2
    oTt = c.view(o, [128, 8, TTA], BF16); o += 8 * TTA * 2
    qit = c.view(o, [64, TTA // 16, 128], BF16); o += TTA * 8 * 2
    wit = c.view(o, [8, TTA], F32); o += TTA * 4
    kblk = [c.view(o + i * 2048, [128, 8, 128], BF16) for i in range(2)]; o += 4096
    vblk = [c.view(o + i * 2048, [128, 1024], BF16) for i in range(2)]; o += 4096
    Rr = [c.view(o + i * 1024, [128, 512], BF16) for i in range(4)]; o += 4096
    selw = c.view(o, [128, 8, 128], BF16); o += 2048
    pT = [c.view(o + i * 2048, [128, 1024], BF16) for i in range(2)]; o += 4096
    identb8 = c.view(o, [128, 1024], BF16); o += 2048
    kit = [c.view(o + i * 1024, [64, 512], BF16) for i in range(2)]; o += 2048
    fixedsel = c.view(o, [128, 8, 128], F32); o += 4096
    headsel = c.view(o, [8, 128], F32); o += 512
    identb = c.view(o, [128, 128], BF16); o += 256
    onesb = c.view(o, [128, 128], BF16); o += 256
    cmask = c.view(o, [128, 512], F32); o += 2048
    relt = c.view(o, [128, 256], F32); o += 1024
    delta = c.view(o, [128, 256], F32); o += 1024
    sm = c.view(o, [128, 16], F32); o += 64
    rl = [c.view(o + i * 512, [128, 128], F32) for i in range(2)]; o += 1024
    rk = c.view(o, [128, 32], F32); o += 128
    pw2 = c.view(o, [128, 32], F32); o += 128
    assert o <= RAW_BYTES, o
    so = 0 if 6 * S >= 25600 else o
    DT = c.view(so, [128, 640], F32)
    Mt = c.view(so + 2560, [128, 640], F32)
    Fg = c.view(so + 5120, [128, 8, 640], F32)
    Xr = [c.bank(0), c.bank(1)]
    sacc = c.bank(2)
    WBps = c.PS[:, 3 * 512: 3 * 512 + 128]
    STw = [c.PS[:, i * 1024:(i + 1) * 1024] for i in range(2)]
    Oacc = [c.PS[:, 4 * 512 + h * 128: 4 * 512 + (h + 1) * 128] for h in range(8)]
    Lacc = [c.PS[:, 6 * 512 + h * 128: 6 * 512 + (h + 1) * 128] for h in range(8)]
    Lh = [c.PS[:, 6 * 512 + i * 512: 6 * 512 + (i + 1) * 512] for i in range(2)]
    coff = sm[:, 15:16]
    p.dma("sp", fixedsel, I["fixedsel"].rearrange("p (j m) -> p j m", j=8), "c_fs", w=["fixedsel"])
    p.dma("sp", headsel, I["headsel"], "c_hs", w=["headsel"])
    p.dma("pool", identb, I["ident"], "c_id", w=["identb"])
    p.dma("sp", cmask, I["cmask"], "c_cm", w=["cmask"])
    p.dma("sp", coff, I["coff"], "c_co", w=["coff"])
    p.op("dve", lambda e: e.memset(onesb, 1.0), w=["onesb"])
    for h in range(8):
        p.op("dve", lambda e, h=h: e.tensor_copy(out=identb8[:, h * 128:(h + 1) * 128], in_=identb), r=["identb"], w=["identb8"])
    for k_ in range(A_NIT + 1):
        p.op("dve", lambda e, k_=k_: e.memset(pw2[:, k_:k_ + 1], 2.0 ** -k_), w=["pw2"])
    load_relt(c, relt, delta, I["relt"])
    for eg in range(A_NEAR // 5):
        p.dma("sp", DT, I["dbase"][:, eg * 640:(eg + 1) * 640], "c_dt", r=["Mt"], w=["DT"])
        p.op("dve", lambda e: e.tensor_scalar(out=DT, in0=DT, scalar1=coff, scalar2=None, op0=ALU.add), r=["DT", "coff"], w=["DT"])
        gen_bias(c, DT, Mt, Fg, relt, delta, 640)
        for h in range(8):
            p.op("dve", lambda e, h=h, eg=eg: e.tensor_scalar(out=btiles[:, eg * 5:(eg + 1) * 5, h, :], in0=Fg[:, h, :].rearrange("p (a b) -> p a b", a=5),
                                                             scalar1=1.0 / A_SCALE, scalar2=None, op0=ALU.mult),
                 r=[f"F{h}"], w=["btiles"])
    for h in range(8):
        p.op("dve", lambda e, h=h: e.tensor_scalar(out=btiles[:, A_NEAR, h, :], in0=cmask[:, 0:128], scalar1=0.0, scalar2=relt[:, 248 + h:249 + h], op0=ALU.mult, op1=ALU.add),
             r=["cmask", "relt"], w=["btiles"])
        p.op("dve", lambda e, h=h: e.tensor_scalar(out=btiles[:, A_NEAR, h, :], in0=btiles[:, A_NEAR, h, :], scalar1=1.0 / A_SCALE, scalar2=None, op0=ALU.mult),
             r=["btiles"], w=["btiles"])
    p.barrier()

    for t in range(T // TTA):
        t0 = t * TTA
        p.dma("sp", qTt, I["qT"][:, :, t0:t0 + TTA].rearrange("h p t -> p h t"), "l_q", w=["qTt"])
        p.dma("sp", qit, I["qiT"][:, t0 * 8:(t0 + TTA) * 8].rearrange("p (a b) -> p a b", b=128), "l_qi", w=["qit"])
        p.dma("sp", wit, I["wiT"][:, t0:t0 + TTA], "l_wi", w=["wit"])
        for s4 in range(TTA // 128):
            s = t * (TTA // 128) + s4
            nkb = CPB * s + CPB
            nk = nkb * 128
            qsl = slice(s4 * 128, (s4 + 1) * 128)
            wb_ps = WBps
            stk = "bk3"
            p.op("pe", lambda e, wb_ps=wb_ps, qsl=qsl: e.matmul(wb_ps, headsel, wit[:, qsl], start=True, stop=True), r=["headsel", "wit"], w=[stk])
            for j in range(8):
                p.op("dve", lambda e, j=j, wb_ps=wb_ps: e.scalar_tensor_tensor(out=selw[:, j, :], in0=fixedsel[:, j, :], scalar=0.125, in1=wb_ps, op0=ALU.mult, op1=ALU.mult),
                     r=["fixedsel", stk], w=["selw"])
            for ck in range(nkb // 4):
                kb = c.nxt("kit")
                p.dma("sp", kit[kb], I["kiT"][:, ck * 512:(ck + 1) * 512], f"l_ki{kb}", w=[f"kit{kb}"])
                for j in range(8):
                    xb = c.nxt("xr")
                    p.op("pe", lambda e, xb=xb, j=j, kb=kb, s4=s4: e.matmul(Xr[xb], qit[:, s4 * 8 + j, :], kit[kb], start=True, stop=True),
                         r=["qit", f"kit{kb}"], w=[f"bk{xb}"])
                    rb = c.nxt("rr", 4)
                    if c.nxt("relu") == 0:
                        p.op("act", lambda e, xb=xb, rb=rb: e.activation(out=Rr[rb], in_=Xr[xb], func=AF.Relu), r=[f"bk{xb}"], w=[f"rr{rb}"])
                    else:
                        p.op("dve", lambda e, xb=xb, rb=rb: e.tensor_scalar(out=Rr[rb], in0=Xr[xb], scalar1=0.0, scalar2=None, op0=ALU.max), r=[f"bk{xb}"], w=[f"rr{rb}"])
                    p.op("pe", lambda e, j=j, rb=rb: e.matmul(sacc, selw[:, j, :], Rr[rb], start=(j == 0), stop=(j == 7)),
                         r=["selw", f"rr{rb}"], w=["bk2"])
                evac(c, scores[:, ck * 512:(ck + 1) * 512], sacc, ["bk2"], ["scores"])
            absmax, lo, mid, cnt, pm5 = (sm[:, i:i + 1] for i in range(5))
            p.op("dve", lambda e, nk=nk: e.tensor_reduce(out=absmax, in_=scores[:, 0:nk], axis=AX.X, op=ALU.max, apply_absolute_value=True),
                 r=["scores"], w=["sm"])
            p.op("dve", lambda e, nk=nk: e.tensor_tensor(out=scores[:, nk - 512:nk], in0=scores[:, nk - 512:nk], in1=cmask, op=ALU.add),
                 r=["scores", "cmask"], w=["scores"])
            p.op("dve", lambda e: e.tensor_scalar(out=rk, in0=pw2, scalar1=absmax, scalar2=None, op0=ALU.mult), r=["sm", "pw2"], w=["rk"])
            p.op("dve", lambda e: e.memset(mid, 0.0), w=["mid"])
            for it in range(A_NIT):
                p.op("dve", lambda e, nk=nk: e.tensor_scalar(out=mterm[:, 0:nk], in0=scores[:, 0:nk], scalar1=mid, scalar2=None, op0=ALU.is_ge, op1=ALU.add, accum_out=cnt),
                     r=["scores", "mid"], w=["mterm", "cnt"])
                p.op("dve", lambda e: e.tensor_scalar(out=pm5, in0=cnt, scalar1=255.5, scalar2=0.5, op0=ALU.is_ge, op1=ALU.subtract), r=["cnt"], w=["pm5"])
                p.op("dve", lambda e, it=it: e.scalar_tensor_tensor(out=mid, in0=pm5, scalar=rk[:, it:it + 1], in1=mid, op0=ALU.mult, op1=ALU.add),
                     r=["pm5", "rk", "mid"], w=["mid"])
            p.op("dve", lambda e: e.scalar_tensor_tensor(out=lo, in0=rk[:, A_NIT:A_NIT + 1], scalar=-1.25, in1=mid, op0=ALU.mult, op1=ALU.add), r=["mid", "rk"], w=["lo"])
            p.op("dve", lambda e, nk=nk: e.tensor_scalar(out=mterm[:, 0:nk], in0=scores[:, 0:nk], scalar1=lo, scalar2=A_MASK, op0=ALU.is_lt, op1=ALU.mult),
                 r=["scores", "lo"], w=["mterm"])
            p.op("dve", lambda e: e.memset(c.PS[:, 4 * 512:8 * 512], 0.0), w=["oacc", "lacc"])
            for j in range(nkb):
                e_ = nkb - 1 - j
                bb = c.nxt("kv")
                p.dma("sp", kblk[bb], I["kTb"][j], f"l_k{bb}", w=[f"kblk{bb}"])
                p.dma("sp", vblk[bb], I["v"][j * 128:(j + 1) * 128, :], f"l_v{bb}", w=[f"vblk{bb}"])
                si = c.nxt("stw")
                st = STw[si]
                bks = [f"bk{2 * si}", f"bk{2 * si + 1}"]
                ei = min(e_, A_NEAR)
                for h in range(8):
                    p.op("pe", lambda e, st=st, bb=bb, h=h, qsl=qsl: e.matmul(st[:, h * 128:(h + 1) * 128], kblk[bb][:, h, :], qTt[:, h, qsl], start=(h % 4 == 0), stop=False, skip_group_check=True),
                         r=[f"kblk{bb}", "qTt"], w=[bks[h // 4]])
                for hf in range(2):
                    p.op("pe", lambda e, st=st, j=j, hf=hf: e.matmul(st[:, hf * 512:(hf + 1) * 512], mterm[:, j * 128:(j + 1) * 128], identb8[:, hf * 512:(hf + 1) * 512], start=False, stop=False, skip_group_check=True),
                         r=["mterm", "identb8"], w=[bks[hf]])
                    p.op("pe", lambda e, st=st, ei=ei, hf=hf: e.matmul(st[:, hf * 512:(hf + 1) * 512], identb, btflat[:, ei, hf * 512:(hf + 1) * 512], start=False, stop=True, skip_group_check=True),
                         r=["btiles", "identb"], w=[bks[hf]])
                pi = c.nxt("pT")
                p.op("act", lambda e, st=st, pi=pi: e.activation(out=pT[pi], in_=st, func=AF.Exp, scale=A_SCALE), r=bks, w=[f"pT{pi}"])
                for h in range(8):
                    p.op("pe", lambda e, bb=bb, h=h, pi=pi: e.matmul(Oacc[h], vblk[bb][:, h * 128:(h + 1) * 128], pT[pi][:, h * 128:(h + 1) * 128], start=False, stop=False, skip_group_check=True),
                         r=[f"vblk{bb}", f"pT{pi}"], w=["oacc"])
                for hf in range(2):
                    p.op("pe", lambda e, hf=hf, pi=pi: e.matmul(Lh[hf], onesb, pT[pi][:, hf * 512:(hf + 1) * 512], start=False, stop=False, skip_group_check=True),
                         r=["onesb", f"pT{pi}"], w=["lacc"])
            if "dbg_sc" in O and s == O.get("dbg_slot", 0):
                p.dma("sp", O["dbg_sc"][:, 0:nk], scores[:, 0:nk], "dbg1", r=["scores"], w=["o_dbg1"])
                p.dma("pool", O["dbg_mt"][:, 0:nk], mterm[:, 0:nk], "dbg2", r=["mterm"], w=["o_dbg2"])
                p.dma("pool", O["dbg_bt"], btiles[:, 0:A_NEAR, 0, :], "dbg3", r=["btiles"], w=["o_dbg3"])
                p.dma("sp", O["dbg_sm"], sm, "dbg4", r=["sm"], w=["o_dbg4"])
                p.op("dve", lambda e: e.tensor_copy(out=DT[:, 0:128], in_=Lacc[0]), r=["lacc"], w=["DTd"])
                p.op("dve", lambda e: e.tensor_copy(out=DT[:, 128:256], in_=Oacc[0]), r=["oacc"], w=["DTd"])
                p.dma("sp", O["dbg_lo"], DT[:, 0:256], "dbg5", r=["DTd"], w=["o_dbg5"])
                p.barrier()
            for h in range(8):
                ri = c.nxt("rl")
                p.op("dve", lambda e, h=h, ri=ri: e.reciprocal(out=rl[ri], in_=Lacc[h]), r=["lacc"], w=[f"rl{ri}"])
                p.op("dve", lambda e, h=h, ri=ri, qsl=qsl: e.tensor_tensor(out=oTt[:, h, qsl], in0=Oacc[h], in1=rl[ri], op=ALU.mult), r=["oacc", f"rl{ri}"], w=["oTt"])
        p.dma("sp", O["oT"][:, :, t0:t0 + TTA].rearrange("h p t -> p h t"), oTt, "st_o", r=["oTt"], w=["o_oT"])


def dram_in(nc, name, shape, dt=F32):
    return nc.dram_tensor(name, list(shape), dt, kind="ExternalInput").ap()


def dram_out(nc, name, shape, dt=F32):
    return nc.dram_tensor(name, list(shape), dt, kind="ExternalOutput").ap()


def build_L1(T):
    nc = bass.Bass("TRN2", target_bir_lowering=False)
    TT = min(1024, T)
    xT = dram_in(nc, "xT", [KC, 128, T])
    gv = dram_in(nc, "gv", [128, 2, KC])
    win = dram_in(nc, "win", [NFF, 128, KC, 256])
    wout = dram_in(nc, "wout", [KC, 128, NFF, 128])
    W = {"wq": dram_in(nc, "wq", [4, 128, KC, 256]), "wk": dram_in(nc, "wk", [4, 128, KC, 256]),
         "wv": dram_in(nc, "wv", [2, 128, KC, 512]), "wqi": dram_in(nc, "wqi", [1, 128, KC, 512]),
         "wkw": dram_in(nc, "wkw", [1, 128, KC, 72])}
    hT = dram_out(nc, "hT", [KC, 128, T])
    O = {"qT": dram_out(nc, "qT", [8, 128, T], BF16), "kTb": dram_out(nc, "kTb", [T // 128, 128, 8, 128], BF16),
         "v": dram_out(nc, "v", [T, 1024], BF16), "qiT": dram_out(nc, "qiT", [64, T * 8], BF16),
         "kiT": dram_out(nc, "kiT", [64, T], BF16), "wiT": dram_out(nc, "wiT", [8, T], F32)}
    c = Ctx(nc, TT)
    c.init_consts()
    load_gcols(c, gv, 2)
    for t0 in range(0, T, TT):
        load_ht(c, xT, t0)
        rmsnorm(c, 0)
        ffn(c, win, wout)
        store_ht(c, hT, t0, key="o_h")
        rmsnorm(c, 1)
        aproj(c, W, O, t0)
    c.p.wait_bufs("sp", ["o_h", "o_q", "o_k", "o_v", "o_qi", "o_ki", "o_wi"])
    c.p.emit()
    return nc, c


def build_L2a(S, T, dbg=None):
    nc = bass.Bass("TRN2", target_bir_lowering=False)
    I = {"qT": dram_in(nc, "qT", [8, 128, T], BF16), "qiT": dram_in(nc, "qiT", [64, T * 8], BF16),
         "wiT": dram_in(nc, "wiT", [8, T], F32), "kTb": dram_in(nc, "kTb", [S // 128, 128, 8, 128], BF16),
         "v": dram_in(nc, "v", [S, 1024], BF16), "kiT": dram_in(nc, "kiT", [64, S], BF16),
         "fixedsel": dram_in(nc, "fixedsel", [128, 1024]), "headsel": dram_in(nc, "headsel", [8, 128]),
         "ident": dram_in(nc, "ident", [128, 128]), "cmask": dram_in(nc, "cmask", [128, 512]),
         "dbase": dram_in(nc, "dbase", [128, A_NEAR * 128]), "coff": dram_in(nc, "coff", [128, 1]),
         "relt": dram_in(nc, "relt", [128, 256])}
    O = {"oT": dram_out(nc, "oT", [8, 128, T], BF16)}
    if dbg is not None:
        O.update({"dbg_slot": dbg, "dbg_sc": dram_out(nc, "dbg_sc", [128, S]), "dbg_mt": dram_out(nc, "dbg_mt", [128, S]),
                  "dbg_bt": dram_out(nc, "dbg_bt", [128, A_NEAR, 128]), "dbg_sm": dram_out(nc, "dbg_sm", [128, 16]),
                  "dbg_lo": dram_out(nc, "dbg_lo", [128, 256])})
    c = Ctx(nc, 512)
    a_attention(c, I, O, S, T)
    c.p.wait_bufs("sp", ["o_oT"])
    c.p.emit()
    return nc, c


def a_consts(cc):
    p = np.arange(128)
    fs = np.zeros((128, 8, 128), np.float32)
    for j in range(8):
        fs[p, j, 16 * j + (p % 16)] = 1.0
    hs = (p[None, :] // 16 == np.arange(8)[:, None]).astype(np.float32)
    q = np.arange(128)[:, None]
    k = np.arange(128)[None, :]
    cm = np.zeros((128, 4, 128), np.float32)
    for b in range(4):
        if b == cc:
            cm[:, b, :] = np.where(k <= q, 0.0, -1e30)
        elif b > cc:
            cm[:, b, :] = -1e30
    kk = np.arange(128)[:, None, None]
    ee = np.arange(A_NEAR)[None, :, None]
    qq = np.arange(128)[None, None, :]
    dbase = (128 * ee + qq - kk).astype(np.float32).reshape(128, A_NEAR * 128)
    return {"fixedsel": fs.reshape(128, 1024), "headsel": hs, "ident": np.eye(128, dtype=np.float32),
            "cmask": cm.reshape(128, 512), "dbase": dbase, "coff": np.full((128, 1), 128.0 * (cc - 3), np.float32)}


def interleave_tokens(xb, cc):
    S = xb.shape[0]
    return np.ascontiguousarray(xb.reshape(S // 128 // CPB, CPB, 128, *xb.shape[1:])[:, cc].reshape(S // CPB, *xb.shape[1:]))


def deinterleave_blocks(per_core):
    a = np.stack(per_core, axis=1)
    return np.ascontiguousarray(a.reshape(a.shape[0] * a.shape[1], *a.shape[2:]))


def linear_in2(c, w_ap, npairs, epi):
    p = c.p
    for pr in range(npairs):
        wb = c.nxt("wgu")
        wgu = c.wgu[wb]
        p.dma("pool", wgu[:, :, 0:256], w_ap[pr], f"wgu{wb}", w=[f"wgu{wb}"])
        for g in range(2):
            ch = pr * 2 + g
            for s in range(c.NS):
                sl = slice(s * 512, (s + 1) * 512)
                b = c.nxt("pm", 4)
                ps = c.pm[b]
                for kc in range(KC):
                    p.op("pe", lambda e, kc=kc, ps=ps, wgu=wgu, sl=sl, g=g: e.matmul(ps, wgu[:, kc, g * 128:(g + 1) * 128], c.xn[:, kc, sl], start=(kc == 0), stop=(kc == KC - 1)),
                         r=[f"xn{kc}", f"wgu{wb}"], w=[f"pm{b}"])
                epi(ch, s, sl, ps, f"pm{b}")


def b_map(c):
    o = c.o_misc
    m = {}
    m["vtm"] = c.view(o, [128, 4, 3072], BF16); o += 24576
    m["vn"] = c.view(o, [128, 3072], BF16); o += 6144
    m["Cc"] = c.view(o, [128, 24, 128], F32); o += 12288
    m["WtT"] = c.view(o, [128, 8, 128], BF16); o += 2048
    m["brow"] = c.view(o, [1, 3072], BF16); o += 6144
    m["lngB"] = c.view(o, [128, 3072], BF16); o += 6144
    m["bucol"] = c.view(o, [128, 24], F32); o += 96
    m["lnbc"] = c.view(o, [128, 24], F32); o += 96
    m["st"] = c.view(o, [128, 8], F32); o += 32
    assert o <= RAW_BYTES, o
    m["bspt"] = c.view(c.o_misc, [128, 8, 128], F32)
    m["tril"] = c.view(c.o_misc + 4096, [128, 128], F32)
    return m


def b_setup(c, W):
    p = c.p
    m = b_map(c)
    p.dma("pool", m["WtT"], W["wspT"].rearrange("p (g t) -> p g t", g=8), "b_wsp", w=["WtT"])
    p.dma("sp", m["tril"], W["trilT"], "b_tril", w=["tril"])
    p.dma("sp", m["bspt"], W["bsp"].rearrange("p (g t) -> p g t", g=8), "b_bsp", w=["bspt"])
    p.dma("pool", m["brow"], W["bv"], "b_bv", w=["brow"])
    p.dma("pool", m["lngB"], W["lng"], "b_lng", w=["lngB"])
    p.dma("sp", m["bucol"], W["bu"], "b_bu", w=["bucol"])
    p.dma("sp", m["lnbc"], W["lnb"], "b_lnb", w=["lnbc"])
    for g in range(8):
        p.op("dve", lambda e, g=g: e.tensor_tensor(out=m["WtT"][:, g, :], in0=m["WtT"][:, g, :], in1=m["tril"], op=ALU.mult), r=["WtT", "tril"], w=["WtT"])
    rs = c.PS[:, 0:1024]
    for g in range(8):
        p.op("pe", lambda e, g=g: e.matmul(rs[:, g * 128:(g + 1) * 128], c.ones, m["WtT"][:, g, :], start=True, stop=True), r=["ones", "WtT"], w=["pm0", "pm1"])
    for j in range(24):
        g = j // 3
        p.op("dve", lambda e, j=j, g=g: e.scalar_tensor_tensor(out=m["Cc"][:, j, :], in0=rs[:, g * 128:(g + 1) * 128], scalar=m["lnbc"][:, j:j + 1], in1=m["bspt"][:, g, :],
                                                            op0=ALU.mult, op1=ALU.add), r=["pm0", "pm1", "lnbc", "bspt"], w=["Cc"])
    p.barrier()
    return m


def mixer_b(c, m, W):
    p = c.p

    def epi_u(ch, s, sl, ps, key):
        p.op("act", lambda e: e.activation(out=c.act[:, ch, sl], in_=ps, func=AF.Gelu_apprx_tanh, bias=m["bucol"][:, ch:ch + 1]), r=[key, "bucol"], w=[f"act{ch}"])

    linear_in2(c, W["wu"], 12, epi_u)
    vtm, vn, st = m["vtm"], m["vn"], m["st"]
    for half in range(c.TT // 512):
        for cg in range(6):
            wb = c.nxt("wgu")
            wgu = c.wgu[wb]
            p.dma("pool", wgu, W["wv"][cg], f"wgu{wb}", w=[f"wgu{wb}"])
            for tc in range(4):
                tok = slice(half * 512 + tc * 128, half * 512 + (tc + 1) * 128)
                b = c.nxt("pm", 4)
                ps = c.pm[b]
                p.op("pe", lambda e, ps=ps, cg=cg: e.matmul(ps, c.ones[0:1, :], m["brow"][0:1, cg * 512:(cg + 1) * 512], start=True, stop=False),
                     r=["ones", "brow"], w=[f"pm{b}"])
                for kc in range(KC):
                    p.op("pe", lambda e, kc=kc, ps=ps, wgu=wgu, tok=tok: e.matmul(ps, c.xn[:, kc, tok], wgu[:, kc, :], start=False, stop=(kc == KC - 1)),
                         r=[f"xn{kc}", f"wgu{wb}"], w=[f"pm{b}"])
                p.op("act", lambda e, ps=ps, tc=tc, cg=cg: e.activation(out=vtm[:, tc, cg * 512:(cg + 1) * 512], in_=ps, func=AF.Gelu_apprx_tanh), r=[f"pm{b}"], w=[f"vtm{tc}"])
        for tc in range(4):
            tok0 = half * 512 + tc * 128
            p.op("dve", lambda e, tc=tc: e.tensor_reduce(out=st[:, 0:1], in_=vtm[:, tc, :], axis=AX.X, op=ALU.add), r=[f"vtm{tc}"], w=["bst0"])
            p.op("dve", lambda e: e.memset(st[:, 1:2], 0.0), w=["bst1"])
            p.op("act", lambda e, tc=tc: e.activation(out=vn, in_=vtm[:, tc, :], func=AF.Square, accum_out=st[:, 1:2]), r=[f"vtm{tc}", "bst1"], w=["vn", "bst1"])
            p.op("dve", lambda e: e.tensor_scalar(out=st[:, 2:3], in0=st[:, 0:1], scalar1=1.0 / 3072, scalar2=None, op0=ALU.mult), r=["bst0"], w=["bst2"])
            p.op("dve", lambda e: e.tensor_tensor(out=st[:, 3:4], in0=st[:, 2:3], in1=st[:, 2:3], op=ALU.mult), r=["bst2"], w=["bst3"])
            p.op("dve", lambda e: e.scalar_tensor_tensor(out=st[:, 4:5], in0=st[:, 1:2], scalar=1.0 / 3072, in1=st[:, 3:4], op0=ALU.mult, op1=ALU.subtract), r=["bst1", "bst3"], w=["bst4"])
            p.op("act", lambda e: e.activation(out=st[:, 5:6], in_=st[:, 4:5], func=AF.Sqrt, bias=EPS, scale=1.0), r=["bst4"], w=["bst5"])
            p.op("dve", lambda e: e.reciprocal(out=st[:, 6:7], in_=st[:, 5:6]), r=["bst5"], w=["bst6"])
            p.op("dve", lambda e, tc=tc: e.tensor_scalar(out=vn, in0=vtm[:, tc, :], scalar1=st[:, 2:3], scalar2=st[:, 6:7], op0=ALU.subtract, op1=ALU.mult),
                 r=[f"vtm{tc}", "bst2", "bst6"], w=["vn"])
            p.op("dve", lambda e: e.tensor_tensor(out=vn, in0=vn, in1=m["lngB"], op=ALU.mult), r=["vn", "lngB"], w=["vn"])
            for j4 in range(6):
                b = c.nxt("pm", 4)
                ps = c.pm[b]
                for jj in range(4):
                    j = j4 * 4 + jj
                    p.op("pe", lambda e, ps=ps, jj=jj, j=j: e.matmul(ps[:, jj * 128:(jj + 1) * 128], vn[:, j * 128:(j + 1) * 128], m["WtT"][:, j // 3, :], start=True, stop=True),
                         r=["vn", "WtT"], w=[f"pm{b}"])
                tb = c.nxt("tmp")
                tmp = c.tmp[tb]
                p.op("dve", lambda e, ps=ps, tmp=tmp, j4=j4: e.tensor_tensor(out=tmp.rearrange("p (a b) -> p a b", a=4), in0=ps.rearrange("p (a b) -> p a b", a=4),
                                                                        in1=m["Cc"][:, j4 * 4:(j4 + 1) * 4, :], op=ALU.add), r=[f"pm{b}", "Cc"], w=[f"tmp{tb}"])
                p.op("dve", lambda e, tmp=tmp, j4=j4, tok0=tok0: e.tensor_tensor(out=c.act[:, j4 * 4:(j4 + 1) * 4, tok0:tok0 + 128], in0=tmp.rearrange("p (a b) -> p a b", a=4),
                                                                            in1=c.act[:, j4 * 4:(j4 + 1) * 4, tok0:tok0 + 128], op=ALU.mult),
                     r=[f"tmp{tb}"] + ACTK[j4 * 4:(j4 + 1) * 4], w=ACTK[j4 * 4:(j4 + 1) * 4])
    linear_out(c, W["wout"], 24, lambda k, sl: c.act[:, k, sl], lambda k: f"act{k}", res_add_epi(c))


def c_glu(c, W, cv, y_out, t0):
    p = c.p
    yv = c.view(c.o_act, [128, KC, c.TT], F32)

    def epi(j, s, sl, pa, pg, ka, kg):
        tb = c.nxt("tmp")
        tmp = c.tmp[tb]
        p.op("act", lambda e: e.activation(out=tmp, in_=pg, func=AF.Sigmoid, bias=cv[:, 1, j:j + 1]), r=[kg, "cvec"], w=[f"tmp{tb}"])
        p.op("dve", lambda e: e.scalar_tensor_tensor(out=yv[:, j, sl], in0=pa, scalar=cv[:, 0, j:j + 1], in1=tmp, op0=ALU.add, op1=ALU.mult),
             r=[ka, f"tmp{tb}", "cvec"], w=ACTK)

    glu_in(c, W["pw1"], 8, epi)
    p.dma("sp", y_out[:, :, t0:t0 + c.TT].rearrange("k p t -> p k t"), yv, "st_y", r=ACTK, w=["o_y"])


def c_conv(c, W, cv, cw, ypad_ap, t0):
    p = c.p
    TT = c.TT
    yp = c.view(c.o_act, [128, KC, 30 + TT], F32)
    acc = c.view(c.o_misc, [128, KC, TT], F32)
    scr = c.view(c.o_misc + KC * TT * 4, [128, 512], F32)
    p.dma("sp", yp, ypad_ap[:, :, t0:t0 + 30 + TT].rearrange("k p t -> p k t"), "l_yp", w=ACTK)
    for kc in range(KC):
        eng = "dve"
        p.op(eng, lambda e, kc=kc: e.tensor_scalar(out=acc[:, kc, :], in0=yp[:, kc, 0:TT], scalar1=cw[:, kc, 0:1], scalar2=cv[:, 2, kc:kc + 1], op0=ALU.mult, op1=ALU.add),
             r=ACTK + ["cvec"], w=[f"acc{kc}"])
        for j in range(1, 31):
            p.op(eng, lambda e, kc=kc, j=j: e.scalar_tensor_tensor(out=acc[:, kc, :], in0=yp[:, kc, j:j + TT], scalar=cw[:, kc, j:j + 1], in1=acc[:, kc, :], op0=ALU.mult, op1=ALU.add),
                 r=["cvec", f"acc{kc}"], w=[f"acc{kc}"])
    ACC = [f"acc{k}" for k in range(KC)]
    p.op("act", lambda e: e.activation(out=c.sq, in_=acc, func=AF.Copy), r=ACC, w=["sq"])
    p.op("act", lambda e: e.activation(out=c.xn, in_=acc, func=AF.Square), r=ACC, w=XN)
    mean = [c.tmp[0], c.tmp[1]]
    rstd = [c.rstd[0], c.rstd[1]]
    for s in range(c.NS):
        sl = slice(s * 512, (s + 1) * 512)
        for kc in range(KC):
            p.op("pe", lambda e, kc=kc, sl=sl: e.matmul(c.pst[0], c.ones, c.sq[:, kc, sl], start=(kc == 0), stop=(kc == KC - 1)), r=["sq", "ones"], w=["pst0"])
        for kc in range(KC):
            p.op("pe", lambda e, kc=kc, sl=sl: e.matmul(c.pst[1], c.ones, c.xn[:, kc, sl], start=(kc == 0), stop=(kc == KC - 1)), r=[f"xn{kc}", "ones"], w=["pst1"])
        p.op("dve", lambda e, s=s: e.tensor_scalar(out=mean[s], in0=c.pst[0], scalar1=1.0 / D, scalar2=None, op0=ALU.mult), r=["pst0"], w=[f"tmp{s}"])
        p.op("dve", lambda e, s=s: e.tensor_tensor(out=scr, in0=mean[s], in1=mean[s], op=ALU.mult), r=[f"tmp{s}"], w=["scr"])
        p.op("dve", lambda e: e.scalar_tensor_tensor(out=scr, in0=c.pst[1], scalar=1.0 / D, in1=scr, op0=ALU.mult, op1=ALU.subtract), r=["pst1", "scr"], w=["scr"])
        p.op("act", lambda e: e.activation(out=scr, in_=scr, func=AF.Sqrt, bias=EPS, scale=1.0), r=["scr"], w=["scr"])
        p.op("dve", lambda e, s=s: e.reciprocal(out=rstd[s], in_=scr), r=["scr"], w=[f"rstd{s}"])
    for s in range(c.NS):
        sl = slice(s * 512, (s + 1) * 512)
        for kc in range(KC):
            eng = "dve" if kc % 2 == 0 else "pool"
            p.op("dve", lambda e, kc=kc, sl=sl, s=s: e.tensor_tensor(out=acc[:, kc, sl], in0=acc[:, kc, sl], in1=mean[s], op=ALU.subtract), r=[f"acc{kc}", f"tmp{s}"], w=[f"acc{kc}"])
            p.op(eng, lambda e, kc=kc, sl=sl, s=s: e.tensor_tensor(out=acc[:, kc, sl], in0=acc[:, kc, sl], in1=rstd[s], op=ALU.mult), r=[f"acc{kc}", f"rstd{s}"], w=[f"acc{kc}"])
            p.op("act", lambda e, kc=kc, sl=sl: e.activation(out=c.xn[:, kc, sl], in_=acc[:, kc, sl], func=AF.Silu, scale=cv[:, 3, kc:kc + 1], bias=cv[:, 4, kc:kc + 1]),
                 r=[f"acc{kc}", "cvec"], w=[f"xn{kc}"])
    linear_out(c, W["pw2"], 8, lambda k, sl: c.xn[:, k, sl], lambda k: f"xn{k}", res_add_epi(c, lambda oc: cv[:, 5, oc:oc + 1]))


def dproj(c, W, O, t0):
    p = c.p
    TT = c.TT
    nb = TT // 128
    vst = c.view(c.o_misc, [128, nb, 1536], BF16)
    ACC = [f"acc{k}" for k in range(KC)]

    def epi(ch, s, sl, ps, key):
        evac(c, c.act[:, ch, sl], ps, [key], [f"act{ch}"])

    linear_in2(c, W["wqk"], 12, epi)
    for cg in range(3):
        wb = c.nxt("wgu")
        wgu = c.wgu[wb]
        p.dma("pool", wgu, W["wv"][cg], f"wgu{wb}", w=[f"wgu{wb}"])
        for tb in range(nb):
            b = c.nxt("pm", 4)
            ps = c.pm[b]
            for kc in range(KC):
                p.op("pe", lambda e, kc=kc, ps=ps, wgu=wgu, tb=tb: e.matmul(ps, c.xn[:, kc, tb * 128:(tb + 1) * 128], wgu[:, kc, :], start=(kc == 0), stop=(kc == KC - 1)),
                     r=[f"xn{kc}", f"wgu{wb}"], w=[f"pm{b}"])
            evac(c, vst[:, tb, cg * 512:(cg + 1) * 512], ps, [f"pm{b}"], ACC)
    p.dma("sp", O["qkT"][:, :, t0:t0 + TT].rearrange("c p t -> p c t"), c.act, "st_qk", r=ACTK, w=["o_qk"])
    p.dma("sp", O["vD"][t0:t0 + TT, :].rearrange("(b p) n -> p b n", p=128), vst, "st_vd", r=ACC, w=["o_vd"])


D_PAIRS = ((128, 1), (512, 4), (2048, 16))
D_SCALE = 64 ** -0.5
D_HALO = 2048


def d_attention(c, I, O, T):
    p = c.p
    SP = 2048
    o = 0
    qs = c.view(o, [128, 2, SP], BF16); o += 2 * SP * 2
    ks = c.view(o, [128, 2, 2 * SP], BF16); o += 4 * SP * 2
    vb = [[c.view(o + (i * 2 + kb) * 512, [128, 256], BF16) for kb in range(2)] for i in range(2)]; o += 2048
    num = c.view(o, [64, 4, SP], F32); o += 4 * SP * 4
    den = c.view(o, [64, 4, SP], F32); o += 4 * SP * 4
    btD = c.view(o, [128, 6, 8, 128], BF16); o += 6 * 8 * 256
    btD0 = c.view(o, [128, 3, 8, 128], BF16); o += 3 * 8 * 256
    pT = [c.view(o + i * 256, [128, 128], BF16) for i in range(4)]; o += 1024
    identb = c.view(o, [128, 128], BF16); o += 256
    onesb = c.view(o, [128, 64], BF16); o += 128
    relt = c.view(o, [128, 256], F32); o += 1024
    delta = c.view(o, [128, 256], F32); o += 1024
    DT = c.view(o, [128, 128], F32); o += 512
    Mt = c.view(o, [128, 128], F32); o += 512
    Fg = c.view(o, [128, 8, 128], F32); o += 4096
    dqk = c.view(o, [128, 128], F32); o += 512
    wm = c.view(o, [128, 2, 128], F32); o += 1024
    negbig = c.view(o, [128, 1], F32); o += 4
    yD = c.view(o, [64, 4, SP], BF16); o += 4 * SP * 2
    rden = [c.view(o + i * 2048, [64, 512], F32) for i in range(2)]; o += 4096
    assert o <= RAW_BYTES, o
    STr = [c.PS[:, i * 128:(i + 1) * 128] for i in range(4)]
    NUMr = [c.PS[0:64, 512 + i * 128: 512 + (i + 1) * 128] for i in range(4)]
    DENr = [c.PS[0:64, 1024 + i * 128: 1024 + (i + 1) * 128] for i in range(4)]
    p.dma("pool", identb, I["ident"], "c_id", w=["identb"])
    p.dma("sp", dqk, I["dqk"], "c_dqk", w=["dqk"])
    p.dma("sp", wm, I["wm"].rearrange("p (a b) -> p a b", a=2), "c_wm", w=["wm"])
    p.dma("sp", negbig, I["negbig"], "c_nb", w=["negbig"])
    p.op("dve", lambda e: e.memset(onesb, 1.0), w=["onesb"])
    load_relt(c, relt, delta, I["relt"])
    for g, (window, dil) in enumerate(D_PAIRS):
        for kb in range(2):
            p.op("dve", lambda e, kb=kb, dil=dil: e.tensor_scalar(out=DT, in0=dqk, scalar1=128.0 * (1 - kb), scalar2=float(dil), op0=ALU.add, op1=ALU.mult),
                 r=["dqk", "Mt"] + [f"F{h}" for h in range(8)], w=["DT"])
            gen_bias(c, DT, Mt, Fg, relt, delta, 128)
            for h in range(8):
                p.op("dve", lambda e, g=g, kb=kb, h=h: e.scalar_tensor_tensor(out=btD[:, g * 2 + kb, h, :], in0=Fg[:, h, :], scalar=1.0 / D_SCALE, in1=wm[:, kb, :], op0=ALU.mult, op1=ALU.add),
                     r=[f"F{h}", "wm"], w=["btD"])
        p.op("dve", lambda e, g=g: e.tensor_scalar(out=btD0[:, g, :, :], in0=btD[:, g * 2, :, :], scalar1=negbig, scalar2=None, op0=ALU.add), r=["btD", "negbig"], w=["btD0"])
    HB = D_HALO
    for sp in range(T // SP):
        for hh in range(2):
            for g, (window, dil) in enumerate(D_PAIRS):
                gspan = 128 * dil
                base = HB + sp * SP
                p.dma("sp", qs, I["qkT"][g * 4 + hh * 2: g * 4 + hh * 2 + 2, :, base:base + SP].rearrange("c p t -> p c t"), "l_qs", w=["qs"])
                p.dma("sp", ks, I["qkT"][12 + g * 4 + hh * 2: 12 + g * 4 + hh * 2 + 2, :, base - SP:base + SP].rearrange("c p t -> p c t"), "l_ks", w=["ks"])
                for u in range(SP // gspan):
                    for r_ in range(dil):
                        q0 = u * gspan + r_
                        qtok = slice(q0, q0 + 127 * dil + 1, dil)
                        vi = c.nxt("vb")
                        for kb in range(2):
                            k0 = SP + q0 - (1 - kb) * gspan
                            row0 = base - SP + k0
                            src = I["vD"][row0: row0 + 127 * dil + 1: dil, g * 512 + hh * 256: g * 512 + hh * 256 + 256]
                            p.dma("sp", vb[vi][kb], src, f"l_vb{vi}{kb}", w=[f"vb{vi}{kb}"])
                        first = (sp == 0 and u == 0)
                        for hl in range(4):
                            h = hh * 4 + hl
                            hp = hl % 2
                            ch = hl // 2
                            ni = c.nxt("nd", 4)
                            for kb in range(2):
                                k0 = SP + q0 - (1 - kb) * gspan
                                ktok = slice(k0, k0 + 127 * dil + 1, dil)
                                si = c.nxt("st", 4)
                                st = STr[si]
                                bt = btD0[:, g, h, :] if (first and kb == 0) else btD[:, g * 2 + kb, h, :]
                                p.op("pe", lambda e, st=st, hp=hp, ch=ch, ktok=ktok, qtok=qtok: e.matmul(st, ks[hp * 64:(hp + 1) * 64, ch, ktok], qs[hp * 64:(hp + 1) * 64, ch, qtok], start=True, stop=False),
                                     r=["ks", "qs"], w=[f"st{si}"])
                                p.op("pe", lambda e, st=st, bt=bt: e.matmul(st, identb, bt, start=False, stop=True), r=["identb", "btD", "btD0"], w=[f"st{si}"])
                                pi = c.nxt("pT", 4)
                                p.op("act", lambda e, st=st, pi=pi: e.activation(out=pT[pi], in_=st, func=AF.Exp, scale=D_SCALE), r=[f"st{si}"], w=[f"pT{pi}"])
                                p.op("pe", lambda e, vi=vi, kb=kb, hl=hl, pi=pi, ni=ni: e.matmul(NUMr[ni], vb[vi][kb][:, hl * 64:(hl + 1) * 64], pT[pi], start=(kb == 0), stop=(kb == 1)),
                                     r=[f"vb{vi}{kb}", f"pT{pi}"], w=[f"num{ni}"])
                                p.op("pe", lambda e, pi=pi, ni=ni, kb=kb: e.matmul(DENr[ni], onesb, pT[pi], start=(kb == 0), stop=(kb == 1)),
                                     r=["onesb", f"pT{pi}"], w=[f"den{ni}"])
                            if g == 0:
                                p.op("dve", lambda e, hl=hl, qtok=qtok, ni=ni: e.tensor_copy(out=num[:, hl, qtok], in_=NUMr[ni]), r=[f"num{ni}"], w=[f"nacc{hl}"])
                                p.op("dve", lambda e, hl=hl, qtok=qtok, ni=ni: e.tensor_copy(out=den[:, hl, qtok], in_=DENr[ni]), r=[f"den{ni}"], w=[f"dacc{hl}"])
                            else:
                                p.op("dve", lambda e, hl=hl, qtok=qtok, ni=ni: e.tensor_tensor(out=num[:, hl, qtok], in0=NUMr[ni], in1=num[:, hl, qtok], op=ALU.add),
                                     r=[f"num{ni}", f"nacc{hl}"], w=[f"nacc{hl}"])
                                p.op("dve", lambda e, hl=hl, qtok=qtok, ni=ni: e.tensor_tensor(out=den[:, hl, qtok], in0=DENr[ni], in1=den[:, hl, qtok], op=ALU.add),
                                     r=[f"den{ni}", f"dacc{hl}"], w=[f"dacc{hl}"])
            for hl in range(4):
                for s in range(SP // 512):
                    sl = slice(s * 512, (s + 1) * 512)
                    ri = c.nxt("rden")
                    p.op("dve", lambda e, hl=hl, sl=sl, ri=ri: e.reciprocal(out=rden[ri], in_=den[:, hl, sl]), r=[f"dacc{hl}"], w=[f"rden{ri}"])
                    p.op("dve", lambda e, hl=hl, sl=sl, ri=ri: e.tensor_tensor(out=yD[:, hl, sl], in0=num[:, hl, sl], in1=rden[ri], op=ALU.mult), r=[f"nacc{hl}", f"rden{ri}"], w=["yD"])
            p.dma("sp", O["yD"][hh * 4:(hh + 1) * 4, :, sp * SP:(sp + 1) * SP].rearrange("h p t -> p h t"), yD, "st_yd", r=["yD"], w=["o_yD"])


def d_out(c, W, yD_ap, t0):
    p = c.p
    yv = c.view(KC * c.TT * 4, [64, 8, c.TT], BF16)
    p.dma("sp", yv, yD_ap[:, :, t0:t0 + c.TT].rearrange("h p t -> p h t"), "l_yd", r=["dram_yD"], w=XN)
    for oc in range(KC):
        wb = c.nxt("wo")
        wo = c.wo[wb]
        p.dma("pool", wo[0:64, 0:8, :], W["dwout"][oc], f"wo{wb}", w=[f"wo{wb}"])
        for s in range(c.NS):
            sl = slice(s * 512, (s + 1) * 512)
            b = c.nxt("po")
            po = c.po[b]
            for k in range(8):
                p.op("pe", lambda e, k=k, po=po, wo=wo, sl=sl: e.matmul(po, wo[0:64, k, :], yv[:, k, sl], start=(k == 0), stop=(k == 7)),
                     r=XN + [f"wo{wb}"], w=[f"po{b}"])
            p.op("dve", lambda e, po=po, oc=oc, sl=sl: e.tensor_tensor(out=c.ht[:, oc, sl], in0=po, in1=c.ht[:, oc, sl], op=ALU.add),
                 r=[f"po{b}", f"ht{oc}"], w=[f"ht{oc}"])


def final_norm(c, gi, out_ap, t0):
    p = c.p
    ov = c.view(c.o_act, [128, KC, c.TT], F32)
    p.op("act", lambda e: e.activation(out=c.sq, in_=c.ht, func=AF.Square), r=HT, w=["sq"])
    for s in range(c.NS):
        sl = slice(s * 512, (s + 1) * 512)
        b = c.nxt("pst")
        pst = c.pst[b]
        for kc in range(KC):
            p.op("pe", lambda e, kc=kc, pst=pst, sl=sl: e.matmul(pst, c.ones, c.sq[:, kc, sl], start=(kc == 0), stop=(kc == KC - 1)), r=["sq", "ones"], w=[f"pst{b}"])
        rb = c.nxt("rstd")
        tb = c.nxt("tmp")
        p.op("act", lambda e, pst=pst, tb=tb: e.activation(out=c.tmp[tb], in_=pst, func=AF.Sqrt, bias=EPS, scale=1.0 / D), r=[f"pst{b}"], w=[f"tmp{tb}"])
        p.op("dve", lambda e, rb=rb, tb=tb: e.reciprocal(out=c.rstd[rb], in_=c.tmp[tb]), r=[f"tmp{tb}"], w=[f"rstd{rb}"])
        for kc in range(KC):
            p.op("dve", lambda e, kc=kc, rb=rb, sl=sl: e.scalar_tensor_tensor(out=ov[:, kc, sl], in0=c.ht[:, kc, sl], scalar=c.gcols[:, gi, kc:kc + 1], in1=c.rstd[rb], op0=ALU.mult, op1=ALU.mult),
                 r=[f"ht{kc}", f"rstd{rb}", "gcols"], w=ACTK)
    p.dma("sp", out_ap[:, :, t0:t0 + c.TT].rearrange("k p t -> p k t"), ov, "st_out", r=ACTK, w=["o_out"])


def ffn_inputs(nc, tag):
    return (dram_in(nc, f"win_{tag}", [NFF, 128, KC, 256]), dram_in(nc, f"wout_{tag}", [KC, 128, NFF, 128]))


def build_L2(S, T):
    nc = bass.Bass("TRN2", target_bir_lowering=False)
    TT = min(1024, T)
    I = {"qT": dram_in(nc, "qT", [8, 128, T], BF16), "qiT": dram_in(nc, "qiT", [64, T * 8], BF16),
         "wiT": dram_in(nc, "wiT", [8, T], F32), "kTb": dram_in(nc, "kTb", [S // 128, 128, 8, 128], BF16),
         "v": dram_in(nc, "v", [S, 1024], BF16), "kiT": dram_in(nc, "kiT", [64, S], BF16),
         "fixedsel": dram_in(nc, "fixedsel", [128, 1024]), "headsel": dram_in(nc, "headsel", [8, 128]),
         "ident": dram_in(nc, "ident", [128, 128]), "cmask": dram_in(nc, "cmask", [128, 512]),
         "dbase": dram_in(nc, "dbase", [128, A_NEAR * 128]), "coff": dram_in(nc, "coff", [128, 1]),
         "relt": dram_in(nc, "relt", [128, 256])}
    oT = nc.dram_tensor("oT_scr", [8, 128, T], BF16, kind="Internal").ap()
    hin = dram_in(nc, "hT_in", [KC, 128, T])
    gv = dram_in(nc, "gv", [128, 6, KC])
    awout = dram_in(nc, "awout", [KC, 128, 8, 128])
    F01 = ffn_inputs(nc, "01"); F10 = ffn_inputs(nc, "10"); F11 = ffn_inputs(nc, "11"); F20 = ffn_inputs(nc, "20")
    WB = {"wu": dram_in(nc, "b_wu", [12, 128, KC, 256]), "wv": dram_in(nc, "b_wv", [6, 128, KC, 512]), "wout": dram_in(nc, "b_wout", [KC, 128, 24, 128]),
          "wspT": dram_in(nc, "b_wspT", [128, 1024]), "trilT": dram_in(nc, "b_trilT", [128, 128]), "bsp": dram_in(nc, "b_bsp", [128, 1024]),
          "bv": dram_in(nc, "b_bv", [1, 3072]), "lng": dram_in(nc, "b_lng", [128, 3072]), "bu": dram_in(nc, "b_bu", [128, 24]), "lnb": dram_in(nc, "b_lnb", [128, 24])}
    WC = {"pw1": dram_in(nc, "c_pw1", [8, 128, KC, 256])}
    cvec = dram_in(nc, "c_vec", [128, 8, 8])
    hout = dram_out(nc, "hT", [KC, 128, T])
    yout = dram_out(nc, "yT", [KC, 128, T])
    c = Ctx(nc, TT)
    a_attention(c, I, {"oT": oT}, S, T)
    c.p.barrier()
    c.init_consts()
    load_gcols(c, gv, 6)
    cv = c.view(c.o_misc + 60000 - 512, [128, 8, 8], F32) if False else None
    m = b_setup(c, WB)
    cvv = c.view(RAW_BYTES - 256, [128, 8, 8], F32)
    c.p.dma("sp", cvv, cvec, "cvec", w=["cvec"])
    for t0 in range(0, T, TT):
        load_ht(c, hin, t0)
        c.p.dma("sp", c.xn, oT[:, :, t0:t0 + TT].rearrange("h p t -> p h t"), "l_oT", r=["o_oT"], w=XN)
        linear_out(c, awout, 8, lambda k, sl: c.xn[:, k, sl], lambda k: f"xn{k}", res_add_epi(c))
        rmsnorm(c, 0); ffn(c, *F01)
        rmsnorm(c, 1); ffn(c, *F10)
        rmsnorm(c, 2); mixer_b(c, m, WB)
        rmsnorm(c, 3); ffn(c, *F11)
        rmsnorm(c, 4); ffn(c, *F20)
        store_ht(c, hout, t0, key="o_h")
        rmsnorm(c, 5)
        c_glu(c, WC, cvv, yout, t0)
    c.p.wait_bufs("sp", ["o_h", "o_y"])
    c.p.emit()
    return nc, c


def build_L3(T):
    nc = bass.Bass("TRN2", target_bir_lowering=False)
    TT = min(1024, T)
    hin = dram_in(nc, "hT_in", [KC, 128, T])
    ypad = dram_in(nc, "ypad", [KC, 128, 30 + T])
    gv = dram_in(nc, "gv", [128, 3, KC])
    cvec = dram_in(nc, "c_vec", [128, 8, 8])
    cwdw = dram_in(nc, "c_wdw", [128, 8, 31])
    WC = {"pw2": dram_in(nc, "c_pw2", [KC, 128, 8, 128])}
    F21 = ffn_inputs(nc, "21"); F30 = ffn_inputs(nc, "30")
    WD = {"wqk": dram_in(nc, "d_wqk", [12, 128, KC, 256]), "wv": dram_in(nc, "d_wv", [3, 128, KC, 512])}
    hout = dram_out(nc, "hT", [KC, 128, T])
    O = {"qkT": dram_out(nc, "qkT", [24, 128, T], BF16), "vD": dram_out(nc, "vD", [T, 1536], BF16)}
    c = Ctx(nc, TT)
    c.init_consts()
    load_gcols(c, gv, 3)
    cvv = c.view(RAW_BYTES - 256, [128, 8, 8], F32)
    cw = c.view(RAW_BYTES - 256 - 992, [128, 8, 31], F32)
    c.p.dma("sp", cvv, cvec, "cvec", w=["cvec"])
    c.p.dma("sp", cw, cwdw, "cvec", w=["cvec"])
    for t0 in range(0, T, TT):
        load_ht(c, hin, t0)
        c_conv(c, WC, cvv, cw, ypad, t0)
        rmsnorm(c, 0); ffn(c, *F21)
        rmsnorm(c, 1); ffn(c, *F30)
        store_ht(c, hout, t0, key="o_h")
        rmsnorm(c, 2)
        dproj(c, WD, O, t0)
    c.p.wait_bufs("sp", ["o_h", "o_qk", "o_vd"])
    c.p.emit()
    return nc, c


def build_L4(T):
    nc = bass.Bass("TRN2", target_bir_lowering=False)
    TT = min(1024, T)
    I = {"qkT": dram_in(nc, "qkT", [24, 128, D_HALO + T], BF16), "vD": dram_in(nc, "vD", [D_HALO + T, 1536], BF16),
         "ident": dram_in(nc, "ident", [128, 128]), "dqk": dram_in(nc, "dqk", [128, 128]), "wm": dram_in(nc, "wm", [128, 256]),
         "negbig": dram_in(nc, "negbig", [128, 1]), "relt": dram_in(nc, "relt", [128, 256])}
    yD = nc.dram_tensor("yD_scr", [8, 64, T], BF16, kind="Internal").ap()
    hin = dram_in(nc, "hT_in", [KC, 128, T])
    gv = dram_in(nc, "gv", [128, 2, KC])
    WD = {"dwout": dram_in(nc, "d_wout", [KC, 64, 8, 128])}
    F31 = ffn_inputs(nc, "31")
    out = dram_out(nc, "outT", [KC, 128, T])
    c = Ctx(nc, TT)
    d_attention(c, I, {"yD": yD}, T)
    c.p.barrier()
    c.init_consts()
    load_gcols(c, gv, 2)
    for t0 in range(0, T, TT):
        load_ht(c, hin, t0)
        d_out(c, WD, yD, t0)
        rmsnorm(c, 0); ffn(c, *F31)
        final_norm(c, 1, out, t0)
    c.p.wait_bufs("sp", ["o_out"])
    c.p.emit()
    return nc, c


_PROGS = {}


def _prog(name, builder, *args):
    key = (name,) + args
    if key not in _PROGS:
        _PROGS[key] = builder(*args)[0]
    return _PROGS[key]


def _rep(v, n=128):
    return np.ascontiguousarray(np.broadcast_to(np.asarray(v, np.float32).reshape(1, -1), (n, v.size)))


def _ffn_w(inp, i, k, tag):
    return {f"win_{tag}": lay_pair(inp["ffn_w_in"][i, k], DFF), f"wout_{tag}": lay_out(inp["ffn_w_out"][i, k])}


def _fm_full(per_core_blocks_interleaved, S):
    a = np.stack([x.reshape(x.shape[0], -1, 128) for x in per_core_blocks_interleaved], axis=2)
    return np.ascontiguousarray(a.reshape(a.shape[0], S))


def run_model(inp, B, S, stages=None, dump=None):
    inp = {k: np.asarray(v, dtype=np.float32) for k, v in inp.items()}
    T = S // CPB
    ncores = B * CPB
    cores = list(range(ncores))
    x = inp["x"]
    ng = inp["norm_g"]
    relt = _rep(inp["rel_table"].reshape(-1))
    ident = np.eye(128, dtype=np.float32)
    aw = inp["a_w_in"][0]
    w1 = {"gv": lay_vec(ng[0, 0:2]),
          "wq": lay_cols(aw[:, 0:1024], 256), "wk": lay_cols(aw[:, 1024:2048], 256), "wv": lay_cols(aw[:, 2048:3072], 512),
          "wqi": lay_cols(aw[:, 3072:3584], 512), "wkw": lay_cols(aw[:, 3584:3656], 72)}
    f00 = _ffn_w(inp, 0, 0, "x")
    w1["win"] = f00["win_x"]; w1["wout"] = f00["wout_x"]
    ims = [dict(w1, xT=lay_xT(interleave_tokens(x[c // CPB], c % CPB))) for c in cores]
    r1 = run_bass_kernel_spmd(_prog("L1", build_L1, T), ims, core_ids=cores).results
    if dump is not None:
        dump["r1"] = r1
    w2 = {"gv": lay_vec(np.stack([ng[0, 2], ng[1, 0], ng[1, 1], ng[1, 2], ng[2, 0], ng[2, 1]])),
          "awout": lay_out(inp["a_w_out"][0]), "relt": relt}
    w2.update(_ffn_w(inp, 0, 1, "01")); w2.update(_ffn_w(inp, 1, 0, "10")); w2.update(_ffn_w(inp, 1, 1, "11")); w2.update(_ffn_w(inp, 2, 0, "20"))
    bw = inp["b_w_in"][0]
    sidx = np.arange(128)
    w2.update({"b_wu": lay_cols(bw[:, :3072], 256), "b_wv": lay_cols(bw[:, 3072:], 512), "b_wout": lay_out(inp["b_w_out"][0]),
               "b_wspT": np.ascontiguousarray(inp["b_w_sp"][0].transpose(2, 0, 1).reshape(128, 1024)),
               "b_trilT": (sidx[:, None] <= sidx[None, :]).astype(np.float32),
               "b_bsp": _rep(inp["b_b_sp"][0].reshape(-1)), "b_bv": np.ascontiguousarray(inp["b_b_in"][0][3072:][None]),
               "b_lng": _rep(inp["b_ln_g"][0]), "b_bu": np.ascontiguousarray(lay_vec(inp["b_b_in"][0][None, :3072])[:, 0, :]),
               "b_lnb": np.ascontiguousarray(lay_vec(inp["b_ln_b"][0][None])[:, 0, :]),
               "c_pw1": lay_pair(inp["c_w_pw1"][0], 1024)})
    cb = inp["c_b_pw1"][0]
    cvec = lay_vec(np.stack([cb[:1024], cb[1024:], inp["c_b_dw"][0], inp["c_ln_g"][0], inp["c_ln_b"][0], inp["c_b_pw2"][0],
                             np.zeros(1024, np.float32), np.zeros(1024, np.float32)]))
    w2["c_vec"] = cvec
    ims = []
    for b in range(B):
        cs = [b * CPB + cc for cc in range(CPB)]
        kTb_all = deinterleave_blocks([np.asarray(r1[c]["kTb"]) for c in cs])
        v_all = deinterleave_blocks([np.asarray(r1[c]["v"]).reshape(T // 128, 128, 1024) for c in cs]).reshape(S, 1024)
        kiT_all = np.ascontiguousarray(np.stack([np.asarray(r1[c]["kiT"]).reshape(64, T // 128, 128) for c in cs], axis=2).reshape(64, S))
        for cc in range(CPB):
            c = cs[cc]
            d = dict(w2, qT=r1[c]["qT"], qiT=r1[c]["qiT"], wiT=r1[c]["wiT"], kTb=kTb_all, v=v_all, kiT=kiT_all, hT_in=r1[c]["hT"])
            d.update(a_consts(cc))
            ims.append(d)
    r2 = run_bass_kernel_spmd(_prog("L2", build_L2, S, T), ims, core_ids=cores).results
    if dump is not None:
        dump["r2"] = r2
    w3 = {"gv": lay_vec(np.stack([ng[2, 2], ng[3, 0], ng[3, 1]])), "c_vec": cvec,
          "c_wdw": np.ascontiguousarray(inp["c_w_dw"][0].T.reshape(KC, 128, 31).transpose(1, 0, 2)),
          "c_pw2": lay_out(inp["c_w_pw2"][0]),
          "d_wqk": lay_cols(inp["d_w_in"][0][:, :3072], 256), "d_wv": lay_cols(inp["d_w_in"][0][:, 3072:], 512)}
    w3.update(_ffn_w(inp, 2, 1, "21")); w3.update(_ffn_w(inp, 3, 0, "30"))
    ims = []
    for b in range(B):
        cs = [b * CPB + cc for cc in range(CPB)]
        hfull = _fm_full([np.asarray(r2[c]["hT"]).reshape(D, T) for c in cs], S)
        yfull = _fm_full([np.asarray(r2[c]["yT"]).reshape(D, T) for c in cs], S)
        ypadfull = np.concatenate([np.zeros((D, 30), np.float32), yfull], axis=1)
        for cc in range(CPB):
            ims.append(dict(w3, hT_in=np.ascontiguousarray(hfull[:, cc * T:(cc + 1) * T]).reshape(KC, 128, T),
                            ypad=np.ascontiguousarray(ypadfull[:, cc * T: cc * T + 30 + T]).reshape(KC, 128, 30 + T)))
    r3 = run_bass_kernel_spmd(_prog("L3", build_L3, T), ims, core_ids=cores).results
    if dump is not None:
        dump["r3"] = r3
    q_ = np.arange(128)[None, :]
    k_ = np.arange(128)[:, None]
    wm = np.concatenate([np.where(k_ >= q_, 0.0, -30000.0), np.where(k_ <= q_, 0.0, -30000.0)], axis=1).astype(np.float32)
    w4 = {"gv": lay_vec(np.stack([ng[3, 2], inp["final_g"]])), "ident": ident, "dqk": (q_ - k_).astype(np.float32), "wm": wm, "relt": relt,
          "d_wout": np.ascontiguousarray(inp["d_w_out"][0].reshape(8, 64, KC, 128).transpose(2, 1, 0, 3))}
    w4.update(_ffn_w(inp, 3, 1, "31"))
    ims = []
    for b in range(B):
        cs = [b * CPB + cc for cc in range(CPB)]
        qk = np.concatenate([np.asarray(r3[c]["qkT"]) for c in cs], axis=2)
        vd = np.concatenate([np.asarray(r3[c]["vD"]) for c in cs], axis=0)
        qk = np.concatenate([np.zeros((24, 128, D_HALO), qk.dtype), qk], axis=2)
        vd = np.concatenate([np.zeros((D_HALO, 1536), vd.dtype), vd], axis=0)
        for cc in range(CPB):
            c = cs[cc]
            ims.append(dict(w4, qkT=np.ascontiguousarray(qk[:, :, cc * T: cc * T + D_HALO + T]), vD=np.ascontiguousarray(vd[cc * T: cc * T + D_HALO + T]),
                            hT_in=r3[c]["hT"], negbig=np.full((128, 1), -30000.0 if cc == 0 else 0.0, np.float32)))
    r4 = run_bass_kernel_spmd(_prog("L4", build_L4, T), ims, core_ids=cores).results
    out = np.empty((B, S, D), np.float32)
    for c in cores:
        out[c // CPB, (c % CPB) * T:((c % CPB) + 1) * T] = unlay_xT(np.asarray(r4[c]["outT"]))
    return out


def kernel(**inputs):
    return run_model(inputs, 2, 16384)
```

```python
import os as _os
import numpy as np
from contextlib import ExitStack
import concourse.bass as bass
import concourse.mybir as mybir
from concourse.bass_utils import run_bass_kernel_spmd

F32 = mybir.dt.float32
BF16 = mybir.dt.bfloat16
ALU = mybir.AluOpType
AF = mybir.ActivationFunctionType
AX = mybir.AxisListType

D = 1024
KC = 8
DFF = 2816
NFF = 22
EPS = 1e-6
NCORES = 8
ENGS = ("pe", "act", "dve", "pool", "sp")
HT = [f"ht{k}" for k in range(8)]
XN = [f"xn{k}" for k in range(8)]


class Prog:
    def __init__(self, nc):
        self.nc = nc
        self.q = {e: [] for e in ENGS}
        self.seen = {e: {} for e in ENGS}
        self.lastw = {}
        self.readers = {}
        self.dmacnt = {}
        self.st = ExitStack()
        self.n_sb = 0

    def sb(self, shape, dt, name=None):
        self.n_sb += 1
        return self.st.enter_context(self.nc.sbuf_tensor(name or f"sb{self.n_sb}", list(shape), dt))

    def ps(self, shape, dt=F32, name=None):
        self.n_sb += 1
        return self.st.enter_context(self.nc.psum_tensor(name or f"ps{self.n_sb}", list(shape), dt))

    def _deps(self, eng, r, w):
        deps = {}

        def add(k, v):
            if deps.get(k, -1) < v:
                deps[k] = v

        for b in r:
            t = self.lastw.get(b)
            if t is not None:
                add(*t)
        for b in w:
            t = self.lastw.get(b)
            if t is not None:
                add(*t)
            for k, v in self.readers.get(b, {}).items():
                add(k, v)
        waits = []
        for k, v in deps.items():
            if k == eng and eng in ("pe", "sp"):
                continue
            if self.seen[eng].get(k, -1) >= v:
                continue
            self.seen[eng][k] = v
            waits.append((k, v))
        return waits

    def _note(self, tok, r, w):
        k, v = tok
        for b in r:
            d = self.readers.setdefault(b, {})
            if d.get(k, -1) < v:
                d[k] = v
        for b in w:
            self.lastw[b] = tok
            self.readers[b] = {}

    def op(self, eng, fn, r=(), w=()):
        waits = self._deps(eng, r, w)
        idx = len(self.q[eng])
        self.q[eng].append((waits, fn, None))
        self._note((eng, idx), r, w)

    def dma(self, queue, out, in_, sem, r=(), w=(), **kw):
        waits = self._deps(queue, r, w)
        key = "d:" + sem
        cum = self.dmacnt.get(key, 0) + 16
        self.dmacnt[key] = cum
        self.q[queue].append((waits, lambda e: e.dma_start(out=out, in_=in_, **kw), key))
        self._note((key, cum), r, w)

    def wait_bufs(self, eng, bufs):
        waits = self._deps(eng, bufs, ())
        self.q[eng].append((waits, None, None))

    def emit(self):
        nc = self.nc
        EP = 60000
        DEP = 3750
        needed = {e: set() for e in ENGS}
        for e in ENGS:
            for waits, fn, dk in self.q[e]:
                for k, v in waits:
                    if k in needed:
                        needed[k].add(v)
        val = {}
        nep = {}
        for e in ENGS:
            c = 0
            m = {}
            for i in range(len(self.q[e])):
                if i in needed[e]:
                    m[i] = (c // EP, c % EP + 1)
                    c += 1
            val[e] = m
            nep[e] = (c + EP - 1) // EP
        sems = {}
        nsem = 0
        for e in ENGS:
            for ep in range(nep[e]):
                sems[(e, ep)] = self.st.enter_context(nc.semaphore(f"s_{e}_{ep}"))
                nsem += 1
        for k in sorted(self.dmacnt.keys()):
            n = self.dmacnt[k] // 16
            for ep in range((n + DEP - 1) // DEP):
                sems[(k, ep)] = self.st.enter_context(nc.semaphore("s_" + k.replace(":", "_") + f"_{ep}"))
                nsem += 1
        block = self.st.enter_context(nc.Block())
        q = self.q
        dcount = {}

        def dsem(k, cum):
            n = cum // 16 - 1
            return sems[(k, n // DEP)], (n % DEP + 1) * 16

        def run(ename, eng):
            for i, (waits, fn, dk) in enumerate(q[ename]):
                for k, v in waits:
                    if k in val:
                        ep, vv = val[k][v]
                        eng.wait_ge(sems[(k, ep)], vv)
                    else:
                        sm, vv = dsem(k, v)
                        eng.wait_ge(sm, vv)
                if fn is None:
                    continue
                ins = fn(eng)
                if dk is not None:
                    dcount[dk] = dcount.get(dk, 0) + 16
                    sm, _ = dsem(dk, dcount[dk])
                    ins.then_inc(sm, 16)
                elif i in val[ename]:
                    ins.then_inc(sems[(ename, val[ename][i][0])], 1)

        block.tensor(lambda eng: run("pe", eng))
        block.scalar(lambda eng: run("act", eng))
        block.vector(lambda eng: run("dve", eng))
        block.gpsimd(lambda eng: run("pool", eng))
        block.sync(lambda eng: run("sp", eng))
        self.st.close()
        self.stats = {e: len(self.q[e]) for e in ENGS}
        self.nsem = nsem


def _barrier(self):
    toks = []
    for e in ENGS:
        for i in range(len(self.q[e]) - 1, -1, -1):
            w_, fn, dk = self.q[e][i]
            if fn is not None and dk is None:
                toks.append((e, i))
                break
    for k, cum in self.dmacnt.items():
        toks.append((k, cum))
    for e in ENGS:
        waits = []
        for k, v in toks:
            if self.seen[e].get(k, -1) >= v:
                continue
            self.seen[e][k] = v
            waits.append((k, v))
        self.q[e].append((waits, None, None))
    self.lastw.clear()
    self.readers.clear()


Prog.barrier = _barrier

RAW_BYTES = 206 * 1024


class Ctx:
    def __init__(self, nc, TT):
        self.nc = nc
        self.p = p = Prog(nc)
        self.TT = TT
        self.NS = TT // 512
        self.RAW = p.sb([128, RAW_BYTES // 2], BF16, "raw")
        self.PS = p.ps([128, 4096], F32, "psum")
        self.cnt = {}
        self.rowlocal_map()

    def view(self, off, shape, dt, p0=0):
        n = int(np.prod(shape[1:]))
        nb = n * (4 if dt == F32 else 2)
        assert off % 4 == 0 and off + nb <= RAW_BYTES, (off, shape)
        a = self.RAW[p0:p0 + shape[0], off // 2: off // 2 + nb // 2]
        if dt == F32:
            a = a.bitcast(F32)
        if len(shape) == 3:
            a = a.rearrange("p (a b) -> p a b", a=shape[1])
        elif len(shape) == 4:
            a = a.rearrange("p (a b c) -> p a b c", a=shape[1], b=shape[2])
        return a

    def bank(self, b):
        return self.PS[:, b * 512:(b + 1) * 512]

    def nxt(self, k, n=2):
        v = self.cnt.get(k, 0)
        self.cnt[k] = v + 1
        return v % n

    def rowlocal_map(self):
        TT = self.TT
        o = 0
        self.ht = self.view(o, [128, KC, TT], F32); o += KC * TT * 4
        self.xn = self.view(o, [128, KC, TT], BF16); o += KC * TT * 2
        self.sq = self.view(o, [128, KC, TT], BF16); self.o_sq = o; o += KC * TT * 2
        self.act = self.view(o, [128, 24, TT], BF16); self.o_act = o; o += 24 * TT * 2
        self.wgu = [self.view(o + i * 8192, [128, KC, 512], BF16) for i in range(2)]; o += 16384
        self.wo = [self.view(o + i * 6144, [128, 24, 128], BF16) for i in range(2)]; o += 12288
        self.tmp = [self.view(o + i * 2048, [128, 512], F32) for i in range(2)]; o += 4096
        self.rstd = [self.view(o + i * 2048, [128, 512], F32) for i in range(2)]; o += 4096
        self.ones = self.view(o, [128, 128], BF16); o += 256
        self.gcols = self.view(o, [128, 16, KC], F32); o += 512
        self.o_misc = o
        self.pm = [self.bank(i) for i in range(4)]
        self.po = [self.bank(4 + i) for i in range(2)]
        self.pst = [self.bank(6 + i) for i in range(2)]

    def init_consts(self):
        self.p.op("dve", lambda e: e.memset(self.ones, 1.0), w=["ones"])


ACTK = [f"act{j}" for j in range(24)]


def load_ht(c, src_ap, t0):
    c.p.dma("sp", c.ht, src_ap[:, :, t0:t0 + c.TT].rearrange("k p t -> p k t"), "ht", r=["dram_h"], w=HT)


def store_ht(c, dst_ap, t0, key="dram_h"):
    c.p.dma("sp", dst_ap[:, :, t0:t0 + c.TT].rearrange("k p t -> p k t"), c.ht, "hst", r=HT, w=[key])


def load_gcols(c, g_ap, n):
    c.p.dma("sp", c.gcols[:, 0:n, :], g_ap, "gcols", w=["gcols"])


def rmsnorm(c, gi):
    p = c.p
    ht, xn, sq = c.ht, c.xn, c.sq
    p.op("act", lambda e: e.activation(out=sq, in_=ht, func=AF.Square), r=HT, w=["sq"])
    for s in range(c.NS):
        sl = slice(s * 512, (s + 1) * 512)
        b = c.nxt("pst")
        pst = c.pst[b]
        for kc in range(KC):
            p.op("pe", lambda e, kc=kc, pst=pst, sl=sl: e.matmul(pst, c.ones, sq[:, kc, sl], start=(kc == 0), stop=(kc == KC - 1)),
                 r=["sq", "ones"], w=[f"pst{b}"])
        rb = c.nxt("rstd")
        rstd = c.rstd[rb]
        tb = c.nxt("tmp")
        tmp = c.tmp[tb]
        p.op("act", lambda e, pst=pst, tmp=tmp: e.activation(out=tmp, in_=pst, func=AF.Sqrt, bias=EPS, scale=1.0 / D),
             r=[f"pst{b}"], w=[f"tmp{tb}"])
        p.op("dve", lambda e, rstd=rstd, tmp=tmp: e.reciprocal(out=rstd, in_=tmp), r=[f"tmp{tb}"], w=[f"rstd{rb}"])
        for kc in range(KC):
            p.op("dve", lambda e, kc=kc, rstd=rstd, sl=sl: e.scalar_tensor_tensor(
                out=xn[:, kc, sl], in0=ht[:, kc, sl], scalar=c.gcols[:, gi, kc:kc + 1], in1=rstd, op0=ALU.mult, op1=ALU.mult),
                r=[f"ht{kc}", f"rstd{rb}", "gcols"], w=[f"xn{kc}"])


def glu_in(c, win_ap, nch, epi):
    p = c.p
    for j in range(nch):
        wb = c.nxt("wgu")
        wgu = c.wgu[wb]
        p.dma("pool", wgu[:, :, 0:256], win_ap[j], f"wgu{wb}", w=[f"wgu{wb}"])
        for s in range(c.NS):
            sl = slice(s * 512, (s + 1) * 512)
            b = c.nxt("pg")
            pg, pu = c.pm[2 * b], c.pm[2 * b + 1]
            for kc in range(KC):
                p.op("pe", lambda e, kc=kc, pg=pg, wgu=wgu, sl=sl: e.matmul(pg, wgu[:, kc, 0:128], c.xn[:, kc, sl], start=(kc == 0), stop=(kc == KC - 1)),
                     r=[f"xn{kc}", f"wgu{wb}"], w=[f"pm{2 * b}"])
            for kc in range(KC):
                p.op("pe", lambda e, kc=kc, pu=pu, wgu=wgu, sl=sl: e.matmul(pu, wgu[:, kc, 128:256], c.xn[:, kc, sl], start=(kc == 0), stop=(kc == KC - 1)),
                     r=[f"xn{kc}", f"wgu{wb}"], w=[f"pm{2 * b + 1}"])
            epi(j, s, sl, pg, pu, f"pm{2 * b}", f"pm{2 * b + 1}")


def linear_out(c, wout_ap, nk, rhs, rkey, epi, n_oc=KC):
    p = c.p
    for oc in range(n_oc):
        wb = c.nxt("wo")
        wo = c.wo[wb]
        p.dma("pool", wo[:, 0:nk, :], wout_ap[oc], f"wo{wb}", w=[f"wo{wb}"])
        for s in range(c.NS):
            sl = slice(s * 512, (s + 1) * 512)
            b = c.nxt("po")
            po = c.po[b]
            for k in range(nk):
                p.op("pe", lambda e, k=k, po=po, wo=wo, sl=sl: e.matmul(po, wo[:, k, :], rhs(k, sl), start=(k == 0), stop=(k == nk - 1)),
                     r=[rkey(k), f"wo{wb}"], w=[f"po{b}"])
            epi(oc, s, sl, po, f"po{b}")


def ffn(c, win_ap, wout_ap):
    p = c.p

    def epi1(j, s, sl, pg, pu, kg, ku):
        tb = c.nxt("tmp")
        tmp = c.tmp[tb]
        p.op("act", lambda e: e.activation(out=tmp, in_=pg, func=AF.Silu), r=[kg], w=[f"tmp{tb}"])
        p.op("dve", lambda e: e.tensor_tensor(out=c.act[:, j, sl], in0=pu, in1=tmp, op=ALU.mult),
             r=[ku, f"tmp{tb}"], w=[f"act{j}"])

    glu_in(c, win_ap, NFF, epi1)

    def epi2(oc, s, sl, po, key):
        p.op("dve", lambda e: e.scalar_tensor_tensor(out=c.ht[:, oc, sl], in0=po, scalar=0.5, in1=c.ht[:, oc, sl], op0=ALU.mult, op1=ALU.add),
             r=[key, f"ht{oc}"], w=[f"ht{oc}"])

    linear_out(c, wout_ap, NFF, lambda k, sl: c.act[:, k, sl], lambda k: f"act{k}", epi2)


def res_add_epi(c, bias_col=None):
    p = c.p

    def epi(oc, s, sl, po, key):
        if bias_col is None:
            p.op("dve", lambda e: e.tensor_tensor(out=c.ht[:, oc, sl], in0=po, in1=c.ht[:, oc, sl], op=ALU.add),
                 r=[key, f"ht{oc}"], w=[f"ht{oc}"])
        else:
            p.op("dve", lambda e: e.scalar_tensor_tensor(out=c.ht[:, oc, sl], in0=po, scalar=bias_col(oc), in1=c.ht[:, oc, sl], op0=ALU.add, op1=ALU.add),
                 r=[key, f"ht{oc}", "cvec"], w=[f"ht{oc}"])
    return epi


def lay_pair(w, half):
    n = half // 128
    a = w[:, :half].reshape(KC, 128, n, 128)
    b = w[:, half:].reshape(KC, 128, n, 128)
    ab = np.concatenate([a, b], axis=3)
    return np.ascontiguousarray(ab.transpose(2, 1, 0, 3))


def lay_out(w):
    nk = w.shape[0] // 128
    no = w.shape[1] // 128
    return np.ascontiguousarray(w.reshape(nk, 128, no, 128).transpose(2, 1, 0, 3))


def lay_cols(w, width):
    n = w.shape[1] // width
    return np.ascontiguousarray(w.reshape(KC, 128, n, width).transpose(2, 1, 0, 3))


def lay_vec(v):
    n = v.shape[0]
    m = v.shape[1] // 128
    return np.ascontiguousarray(v.reshape(n, m, 128).transpose(2, 0, 1))


def lay_xT(x):
    return np.ascontiguousarray(x.T.reshape(KC, 128, x.shape[0]))


def unlay_xT(xt):
    return np.ascontiguousarray(xt.reshape(-1, xt.shape[2]).T)


def evac(c, out, in_, rk, wk, scale=None):
    p = c.p
    if c.nxt("evac") == 0:
        if scale is None:
            p.op("act", lambda e: e.activation(out=out, in_=in_, func=AF.Copy), r=rk, w=wk)
        else:
            p.op("act", lambda e: e.activation(out=out, in_=in_, func=AF.Copy, scale=scale), r=rk, w=wk)
    else:
        if scale is None:
            p.op("dve", lambda e: e.tensor_copy(out=out, in_=in_), r=rk, w=wk)
        else:
            p.op("dve", lambda e: e.tensor_scalar(out=out, in0=in_, scalar1=scale, scalar2=None, op0=ALU.mult), r=rk, w=wk)


def aproj(c, W, O, t0):
    p = c.p
    TT = c.TT
    nb = TT // 128
    qst = c.act[:, 0:8, :]
    kst = c.act[:, 8:16, :]
    qist = c.view(c.o_act + 16 * TT * 2, [64, TT // 16, 128], BF16)
    vst = c.view(c.o_sq, [128, nb, 1024], BF16)
    kist = c.view(c.o_misc, [64, TT], BF16)
    wist = c.view(c.o_misc + 2 * TT, [8, TT], F32)
    for which, wap, st, k0 in (("q", W["wq"], qst, 0), ("k", W["wk"], kst, 8)):
        for g2 in range(4):
            wb = c.nxt("wgu")
            wgu = c.wgu[wb]
            p.dma("pool", wgu[:, :, 0:256], wap[g2], f"wgu{wb}", w=[f"wgu{wb}"])
            for g in range(2):
                ch = g2 * 2 + g
                for s in range(c.NS):
                    sl = slice(s * 512, (s + 1) * 512)
                    b = c.nxt("pm", 4)
                    ps = c.pm[b]
                    for kc in range(KC):
                        p.op("pe", lambda e, kc=kc, ps=ps, wgu=wgu, sl=sl, g=g: e.matmul(ps, wgu[:, kc, g * 128:(g + 1) * 128], c.xn[:, kc, sl], start=(kc == 0), stop=(kc == KC - 1)),
                             r=[f"xn{kc}", f"wgu{wb}"], w=[f"pm{b}"])
                    evac(c, st[:, ch, sl], ps, [f"pm{b}"], [f"act{k0 + ch}"])
    for cg in range(2):
        wb = c.nxt("wgu")
        wgu = c.wgu[wb]
        p.dma("pool", wgu, W["wv"][cg], f"wgu{wb}", w=[f"wgu{wb}"])
        for tb in range(nb):
            b = c.nxt("pm", 4)
            ps = c.pm[b]
            for kc in range(KC):
                p.op("pe", lambda e, kc=kc, ps=ps, wgu=wgu, tb=tb: e.matmul(ps, c.xn[:, kc, tb * 128:(tb + 1) * 128], wgu[:, kc, :], start=(kc == 0), stop=(kc == KC - 1)),
                     r=[f"xn{kc}", f"wgu{wb}"], w=[f"pm{b}"])
            evac(c, vst[:, tb, cg * 512:(cg + 1) * 512], ps, [f"pm{b}"], ["sq"])
    wb = c.nxt("wgu")
    wgu = c.wgu[wb]
    p.dma("pool", wgu, W["wqi"][0], f"wgu{wb}", w=[f"wgu{wb}"])
    for h in range(8):
        for s in range(c.NS):
            sl = slice(s * 512, (s + 1) * 512)
            b = c.nxt("pm", 4)
            ps = c.pm[b]
            for kc in range(KC):
                p.op("pe", lambda e, kc=kc, ps=ps, wgu=wgu, sl=sl, h=h: e.matmul(ps[0:64, :], wgu[:, kc, h * 64:(h + 1) * 64], c.xn[:, kc, sl], start=(kc == 0), stop=(kc == KC - 1)),
                     r=[f"xn{kc}", f"wgu{wb}"], w=[f"pm{b}"])
            evac(c, qist[:, s * 32:(s + 1) * 32, h * 16:(h + 1) * 16], ps[0:64, :].rearrange("p (j q) -> p j q", q=16), [f"pm{b}"], [f"act{16 + hh}" for hh in range(8)])
    wb = c.nxt("wgu")
    wgu = c.wgu[wb]
    p.dma("pool", wgu[:, :, 0:72], W["wkw"][0], f"wgu{wb}", w=[f"wgu{wb}"])
    for s in range(c.NS):
        sl = slice(s * 512, (s + 1) * 512)
        b = c.nxt("pm", 4)
        ps = c.pm[b]
        for kc in range(KC):
            p.op("pe", lambda e, kc=kc, ps=ps, wgu=wgu, sl=sl: e.matmul(ps[0:64, :], wgu[:, kc, 0:64], c.xn[:, kc, sl], start=(kc == 0), stop=(kc == KC - 1)),
                 r=[f"xn{kc}", f"wgu{wb}"], w=[f"pm{b}"])
        evac(c, kist[:, sl], ps[0:64, :], [f"pm{b}"], ["kist"])
        b = c.nxt("pm", 4)
        ps = c.pm[b]
        for kc in range(KC):
            p.op("pe", lambda e, kc=kc, ps=ps, wgu=wgu, sl=sl: e.matmul(ps[0:8, :], wgu[:, kc, 64:72], c.xn[:, kc, sl], start=(kc == 0), stop=(kc == KC - 1)),
                 r=[f"xn{kc}", f"wgu{wb}"], w=[f"pm{b}"])
        evac(c, wist[:, sl], ps[0:8, :], [f"pm{b}"], ["wist"], scale=8 ** -0.5)
    p.dma("sp", O["qT"][:, :, t0:t0 + TT].rearrange("h p t -> p h t"), qst, "st_q", r=ACTK[0:8], w=["o_q"])
    for b_ in range(nb):
        p.dma("sp", O["kTb"][t0 // 128 + b_], kst[:, :, b_ * 128:(b_ + 1) * 128], "st_k", r=ACTK[8:16], w=["o_k"])
    p.dma("sp", O["v"][t0:t0 + TT, :].rearrange("(b p) n -> p b n", p=128), vst, "st_v", r=["sq"], w=["o_v"])
    p.dma("sp", O["qiT"][:, t0 * 8:(t0 + TT) * 8], qist.rearrange("p a b -> p (a b)"), "st_qi", r=ACTK[16:24], w=["o_qi"])
    p.dma("sp", O["kiT"][:, t0:t0 + TT], kist, "st_ki", r=["kist"], w=["o_ki"])
    p.dma("sp", O["wiT"][:, t0:t0 + TT], wist, "st_wi", r=["wist"], w=["o_wi"])


def rel_thresholds():
    d = np.arange(0, 4096, dtype=np.int64)
    df = np.maximum(d, 1).astype(np.float32)
    large = 16 + (np.log(df / np.float32(16)) / np.float32(np.log(2048 / 16)) * np.float32(16)).astype(np.int32)
    large = np.minimum(large, 31)
    bucket = np.where(d < 16, d, large)
    return [int(np.argmax(bucket >= b)) for b in range(1, 32)]


def gen_bias(c, DT, Mt, F, relt, delta, ncols, eng="dve"):
    p = c.p
    thr = rel_thresholds()
    for h in range(8):
        p.op(eng, lambda e, h=h: e.tensor_scalar(out=F[:, h, 0:ncols], in0=DT[:, 0:ncols], scalar1=0.0, scalar2=relt[:, h:h + 1], op0=ALU.mult, op1=ALU.add),
             r=["DT", "relt"], w=[f"F{h}"])
    for b in range(1, 32):
        p.op(eng, lambda e, b=b: e.tensor_scalar(out=Mt[:, 0:ncols], in0=DT[:, 0:ncols], scalar1=float(thr[b - 1]), scalar2=None, op0=ALU.is_ge),
             r=["DT"], w=["Mt"])
        for h in range(8):
            p.op(eng, lambda e, b=b, h=h: e.scalar_tensor_tensor(out=F[:, h, 0:ncols], in0=Mt[:, 0:ncols], scalar=delta[:, b * 8 + h:b * 8 + h + 1],
                                                                in1=F[:, h, 0:ncols], op0=ALU.mult, op1=ALU.add),
                 r=["Mt", "delta", f"F{h}"], w=[f"F{h}"])


def load_relt(c, relt, delta, relt_ap):
    p = c.p
    p.dma("sp", relt, relt_ap, "relt", w=["relt"])
    p.op("dve", lambda e: e.memset(delta[:, 0:8], 0.0), w=["delta"])
    p.op("dve", lambda e: e.tensor_tensor(out=delta[:, 8:256], in0=relt[:, 8:256], in1=relt[:, 0:248], op=ALU.subtract), r=["relt", "delta"], w=["delta"])


A_NIT = 16
A_NEAR = 20
A_MASK = -30000.0
A_SCALE = 128 ** -0.5
CPB = 4


def a_attention(c, I, O, S, T):
    p = c.p
    TTA = int(_os.environ.get('TTA', '256'))
    nslots = T // 128
    o = 0
    scores = c.view(o, [128, S], F32); o += 4 * S
    mterm = c.view(o, [128, S], BF16); o += 2 * S
    btiles = c.view(o, [128, A_NEAR + 1, 8, 128], BF16)
    btflat = c.view(o, [128, A_NEAR + 1, 1024], BF16); o += (A_NEAR + 1) * 8 * 128 * 2
    qTt = c.view(o, [128, 8, TTA], BF16); o += 8 * TTA * 2
    oTt = c.view(o, [128, 8, TTA], BF16); o += 8 * TTA * 2
    qit = c.view(o, [64, TTA // 16, 128], BF16); o += TTA * 8 * 2
    wit = c.view(o, [8, TTA], F32); o += TTA * 4
    kblk = [c.view(o + i * 2048, [128, 8, 128], BF16) for i in range(2)]; o += 4096
    vblk = [c.view(o + i * 2048, [128, 1024], BF16) for i in range(2)]; o += 4096
    Rr = [c.view(o + i * 1024, [128, 512], BF16) for i in range(4)]; o += 4096
    selw = c.view(o, [128, 8, 128], BF16); o += 2048
    pT = [c.view(o + i * 2048, [128, 1024], BF16) for i in range(2)]; o += 4096
    identb8 = c.view(o, [128, 1024], BF16); o += 2048
    kit = [c.view(o + i * 1024, [64, 512], BF16) for i in range(2)]; o += 2048
    fixedsel = c.view(o, [128, 8, 128], F32); o += 4096
    headsel = c.view(o, [8, 128], F32); o += 512
    identb = c.view(o, [128, 128], BF16); o += 256
    onesb = c.view(o, [128, 128], BF16); o += 256
    cmask = c.view(o, [128, 512], F32); o += 2048
    relt = c.view(o, [128, 256], F32); o += 1024
    delta = c.view(o, [128, 256], F32); o += 1024
    sm = c.view(o, [128, 16], F32); o += 64
    rl = [c.view(o + i * 512, [128, 128], F32) for i in range(2)]; o += 1024
    rk = c.view(o, [128, 32], F32); o += 128
    pw2 = c.view(o, [128, 32], F32); o += 128
    if _os.environ.get('JUNKBF'):
        junk = c.view(o, [128, S], BF16); o += 2 * S
    else:
        junk = c.RAW[:, o // 2: o // 2 + S // 2].bitcast(mybir.dt.uint8); o += S
    assert o <= RAW_BYTES, o
    so = 0 if 6 * S >= 25600 else o
    DT = c.view(so, [128, 640], F32)
    Mt = c.view(so + 2560, [128, 640], F32)
    Fg = c.view(so + 5120, [128, 8, 640], F32)
    Xr = [c.bank(0), c.bank(1)]
    sacc = c.bank(2)
    WBps = c.PS[:, 3 * 512: 3 * 512 + 128]
    STw = [c.PS[:, i * 1024:(i + 1) * 1024] for i in range(2)]
    Oacc = [c.PS[:, 4 * 512 + h * 128: 4 * 512 + (h + 1) * 128] for h in range(8)]
    Lacc = [c.PS[:, 6 * 512 + h * 128: 6 * 512 + (h + 1) * 128] for h in range(8)]
    Lh = [c.PS[:, 6 * 512 + i * 512: 6 * 512 + (i + 1) * 512] for i in range(2)]
    coff = sm[:, 15:16]
    p.dma("sp", fixedsel, I["fixedsel"].rearrange("p (j m) -> p j m", j=8), "c_fs", w=["fixedsel"])
    p.dma("sp", headsel, I["headsel"], "c_hs", w=["headsel"])
    p.dma("pool", identb, I["ident"], "c_id", w=["identb"])
    p.dma("sp", cmask, I["cmask"], "c_cm", w=["cmask"])
    p.dma("sp", coff, I["coff"], "c_co", w=["coff"])
    p.op("dve", lambda e: e.memset(onesb, 1.0), w=["onesb"])
    for h in range(8):
        p.op("dve", lambda e, h=h: e.tensor_copy(out=identb8[:, h * 128:(h + 1) * 128], in_=identb), r=["identb"], w=["identb8"])
    for k_ in range(A_NIT + 1):
        p.op("dve", lambda e, k_=k_: e.memset(pw2[:, k_:k_ + 1], 2.0 ** -k_), w=["pw2"])
    load_relt(c, relt, delta, I["relt"])
    for eg in range(A_NEAR // 5):
        p.dma("sp", DT, I["dbase"][:, eg * 640:(eg + 1) * 640], "c_dt", r=["Mt"], w=["DT"])
        p.op("dve", lambda e: e.tensor_scalar(out=DT, in0=DT, scalar1=coff, scalar2=None, op0=ALU.add), r=["DT", "coff"], w=["DT"])
        gen_bias(c, DT, Mt, Fg, relt, delta, 640)
        for h in range(8):
            p.op("dve", lambda e, h=h, eg=eg: e.tensor_scalar(out=btiles[:, eg * 5:(eg + 1) * 5, h, :], in0=Fg[:, h, :].rearrange("p (a b) -> p a b", a=5),
                                                             scalar1=1.0 / A_SCALE, scalar2=None, op0=ALU.mult),
                 r=[f"F{h}"], w=["btiles"])
    for h in range(8):
        p.op("dve", lambda e, h=h: e.tensor_scalar(out=btiles[:, A_NEAR, h, :], in0=cmask[:, 0:128], scalar1=0.0, scalar2=relt[:, 248 + h:249 + h], op0=ALU.mult, op1=ALU.add),
             r=["cmask", "relt"], w=["btiles"])
        p.op("dve", lambda e, h=h: e.tensor_scalar(out=btiles[:, A_NEAR, h, :], in0=btiles[:, A_NEAR, h, :], scalar1=1.0 / A_SCALE, scalar2=None, op0=ALU.mult),
             r=["btiles"], w=["btiles"])
    p.barrier()

    NSL = TTA // 128
    absmax, lo, mid, cnt, pm5 = (sm[:, i:i + 1] for i in range(5))

    def stage_idx(s):
        t, s4 = divmod(s, NSL)
        t0 = t * TTA
        nkb = CPB * s + CPB
        nk = nkb * 128
        qsl = slice(s4 * 128, (s4 + 1) * 128)
        if s4 == 0:
            p.dma("sp", qit, I["qiT"][:, t0 * 8:(t0 + TTA) * 8].rearrange("p (a b) -> p a b", b=128), "l_qi", w=["qit"])
            p.dma("sp", wit, I["wiT"][:, t0:t0 + TTA], "l_wi", w=["wit"])
        p.op("pe", lambda e: e.matmul(WBps, headsel, wit[:, qsl], start=True, stop=True), r=["headsel", "wit"], w=["bk3"])
        for j in range(8):
            p.op("dve", lambda e, j=j: e.scalar_tensor_tensor(out=selw[:, j, :], in0=fixedsel[:, j, :], scalar=0.125, in1=WBps, op0=ALU.mult, op1=ALU.mult),
                 r=["fixedsel", "bk3"], w=["selw"])
        for ck in range(nkb // 4):
            kb = c.nxt("kit")
            p.dma("sp", kit[kb], I["kiT"][:, ck * 512:(ck + 1) * 512], f"l_ki{kb}", w=[f"kit{kb}"])
            for j in range(8):
                xb = c.nxt("xr")
                p.op("pe", lambda e, xb=xb, j=j, kb=kb: e.matmul(Xr[xb], qit[:, s4 * 8 + j, :], kit[kb], start=True, stop=True),
                     r=["qit", f"kit{kb}"], w=[f"bk{xb}"])
                rb = c.nxt("rr", 4)
                if c.nxt("relu") == 0:
                    p.op("act", lambda e, xb=xb, rb=rb: e.activation(out=Rr[rb], in_=Xr[xb], func=AF.Relu), r=[f"bk{xb}"], w=[f"rr{rb}"])
                else:
                    p.op("dve", lambda e, xb=xb, rb=rb: e.tensor_scalar(out=Rr[rb], in0=Xr[xb], scalar1=0.0, scalar2=None, op0=ALU.max), r=[f"bk{xb}"], w=[f"rr{rb}"])
                p.op("pe", lambda e, j=j, rb=rb: e.matmul(sacc, selw[:, j, :], Rr[rb], start=(j == 0), stop=(j == 7)),
                     r=["selw", f"rr{rb}"], w=["bk2"])
            evac(c, scores[:, ck * 512:(ck + 1) * 512], sacc, ["bk2"], ["scores"])
        p.op("dve", lambda e: e.tensor_reduce(out=absmax, in_=scores[:, 0:nk], axis=AX.X, op=ALU.max, apply_absolute_value=True),
             r=["scores"], w=["sm"])
        p.op("dve", lambda e: e.tensor_tensor(out=scores[:, nk - 512:nk], in0=scores[:, nk - 512:nk], in1=cmask, op=ALU.add),
             r=["scores", "cmask"], w=["scores"])
        p.op("dve", lambda e: e.tensor_scalar(out=rk, in0=pw2, scalar1=absmax, scalar2=None, op0=ALU.mult), r=["sm", "pw2"], w=["rk"])
        p.op("dve", lambda e: e.memset(mid, 0.0), w=["mid"])

    def stage_bis(s):
        nk = (CPB * s + CPB) * 128
        for it in range(A_NIT):
            p.op("dve", lambda e: e.tensor_scalar(out=junk[:, 0:nk], in0=scores[:, 0:nk], scalar1=mid, scalar2=None, op0=ALU.is_ge, op1=ALU.add, accum_out=cnt),
                 r=["scores", "mid"], w=["junk", "cnt"])
            p.op("dve", lambda e: e.tensor_scalar(out=pm5, in0=cnt, scalar1=255.5, scalar2=0.5, op0=ALU.is_ge, op1=ALU.subtract), r=["cnt"], w=["pm5"])
            p.op("dve", lambda e, it=it: e.scalar_tensor_tensor(out=mid, in0=pm5, scalar=rk[:, it:it + 1], in1=mid, op0=ALU.mult, op1=ALU.add),
                 r=["pm5", "rk", "mid"], w=["mid"])
        p.op("dve", lambda e: e.scalar_tensor_tensor(out=lo, in0=rk[:, A_NIT:A_NIT + 1], scalar=-1.25, in1=mid, op0=ALU.mult, op1=ALU.add), r=["mid", "rk"], w=["lo"])

    def stage_mask(s):
        nk = (CPB * s + CPB) * 128
        p.op("dve", lambda e: e.tensor_scalar(out=mterm[:, 0:nk], in0=scores[:, 0:nk], scalar1=lo, scalar2=A_MASK, op0=ALU.is_lt, op1=ALU.mult),
             r=["scores", "lo"], w=["mterm"])

    def stage_zero(s):
        p.op("dve", lambda e: e.memset(c.PS[:, 4 * 512:8 * 512], 0.0), w=["oacc", "lacc"])

    def stage_att(s):
        t, s4 = divmod(s, NSL)
        t0 = t * TTA
        nkb = CPB * s + CPB
        qsl = slice(s4 * 128, (s4 + 1) * 128)
        if s4 == 0:
            p.dma("sp", qTt, I["qT"][:, :, t0:t0 + TTA].rearrange("h p t -> p h t"), "l_q", w=["qTt"])
        for j in range(nkb):
            e_ = nkb - 1 - j
            bb = c.nxt("kv")
            p.dma("sp", kblk[bb], I["kTb"][j], f"l_k{bb}", w=[f"kblk{bb}"])
            p.dma("sp", vblk[bb], I["v"][j * 128:(j + 1) * 128, :], f"l_v{bb}", w=[f"vblk{bb}"])
            si = c.nxt("stw")
            st = STw[si]
            bks = [f"bk{2 * si}", f"bk{2 * si + 1}"]
            ei = min(e_, A_NEAR)
            for h in range(8):
                p.op("pe", lambda e, st=st, bb=bb, h=h: e.matmul(st[:, h * 128:(h + 1) * 128], kblk[bb][:, h, :], qTt[:, h, qsl], start=(h % 4 == 0), stop=False, skip_group_check=True),
                     r=[f"kblk{bb}", "qTt"], w=[bks[h // 4]])
            for hf in range(2):
                p.op("pe", lambda e, st=st, j=j, hf=hf: e.matmul(st[:, hf * 512:(hf + 1) * 512], mterm[:, j * 128:(j + 1) * 128], identb8[:, hf * 512:(hf + 1) * 512], start=False, stop=False, skip_group_check=True),
                     r=["mterm", "identb8"], w=[bks[hf]])
                p.op("pe", lambda e, st=st, ei=ei, hf=hf: e.matmul(st[:, hf * 512:(hf + 1) * 512], identb, btflat[:, ei, hf * 512:(hf + 1) * 512], start=False, stop=True, skip_group_check=True),
                     r=["btiles", "identb"], w=[bks[hf]])
            pi = c.nxt("pT")
            p.op("act", lambda e, st=st, pi=pi: e.activation(out=pT[pi], in_=st, func=AF.Exp, scale=A_SCALE), r=bks, w=[f"pT{pi}"])
            for h in range(8):
                p.op("pe", lambda e, bb=bb, h=h, pi=pi: e.matmul(Oacc[h], vblk[bb][:, h * 128:(h + 1) * 128], pT[pi][:, h * 128:(h + 1) * 128], start=False, stop=False, skip_group_check=True),
                     r=[f"vblk{bb}", f"pT{pi}"], w=["oacc"])
            for hf in range(2):
                p.op("pe", lambda e, hf=hf, pi=pi: e.matmul(Lh[hf], onesb, pT[pi][:, hf * 512:(hf + 1) * 512], start=False, stop=False, skip_group_check=True),
                     r=["onesb", f"pT{pi}"], w=["lacc"])

    def stage_norm(s):
        t, s4 = divmod(s, NSL)
        t0 = t * TTA
        qsl = slice(s4 * 128, (s4 + 1) * 128)
        for h in range(8):
            ri = c.nxt("rl")
            p.op("dve", lambda e, h=h, ri=ri: e.reciprocal(out=rl[ri], in_=Lacc[h]), r=["lacc"], w=[f"rl{ri}"])
            p.op("dve", lambda e, h=h, ri=ri: e.tensor_tensor(out=oTt[:, h, qsl], in0=Oacc[h], in1=rl[ri], op=ALU.mult), r=["oacc", f"rl{ri}"], w=["oTt"])
        if s4 == NSL - 1:
            p.dma("sp", O["oT"][:, :, t0:t0 + TTA].rearrange("h p t -> p h t"), oTt, "st_o", r=["oTt"], w=["o_oT"])

    stage_idx(0)
    stage_bis(0)
    for s in range(nslots):
        stage_mask(s)
        if s + 1 < nslots:
            stage_idx(s + 1)
        stage_zero(s)
        if s + 1 < nslots:
            stage_bis(s + 1)
        stage_att(s)
        stage_norm(s)


def dram_in(nc, name, shape, dt=F32):
    return nc.dram_tensor(name, list(shape), dt, kind="ExternalInput").ap()


def dram_out(nc, name, shape, dt=F32):
    return nc.dram_tensor(name, list(shape), dt, kind="ExternalOutput").ap()


def build_L1(T):
    nc = bass.Bass("TRN2", target_bir_lowering=False)
    TT = min(1024, T)
    xT = dram_in(nc, "xT", [KC, 128, T])
    gv = dram_in(nc, "gv", [128, 2, KC])
    win = dram_in(nc, "win", [NFF, 128, KC, 256])
    wout = dram_in(nc, "wout", [KC, 128, NFF, 128])
    W = {"wq": dram_in(nc, "wq", [4, 128, KC, 256]), "wk": dram_in(nc, "wk", [4, 128, KC, 256]),
         "wv": dram_in(nc, "wv", [2, 128, KC, 512]), "wqi": dram_in(nc, "wqi", [1, 128, KC, 512]),
         "wkw": dram_in(nc, "wkw", [1, 128, KC, 72])}
    hT = dram_out(nc, "hT", [KC, 128, T])
    O = {"qT": dram_out(nc, "qT", [8, 128, T], BF16), "kTb": dram_out(nc, "kTb", [T // 128, 128, 8, 128], BF16),
         "v": dram_out(nc, "v", [T, 1024], BF16), "qiT": dram_out(nc, "qiT", [64, T * 8], BF16),
         "kiT": dram_out(nc, "kiT", [64, T], BF16), "wiT": dram_out(nc, "wiT", [8, T], F32)}
    c = Ctx(nc, TT)
    c.init_consts()
    load_gcols(c, gv, 2)
    for t0 in range(0, T, TT):
        load_ht(c, xT, t0)
        rmsnorm(c, 0)
        ffn(c, win, wout)
        store_ht(c, hT, t0, key="o_h")
        rmsnorm(c, 1)
        aproj(c, W, O, t0)
    c.p.wait_bufs("sp", ["o_h", "o_q", "o_k", "o_v", "o_qi", "o_ki", "o_wi"])
    c.p.emit()
    return nc, c


def build_L2a(S, T, dbg=None):
    nc = bass.Bass("TRN2", target_bir_lowering=False)
    I = {"qT": dram_in(nc, "qT", [8, 128, T], BF16), "qiT": dram_in(nc, "qiT", [64, T * 8], BF16),
         "wiT": dram_in(nc, "wiT", [8, T], F32), "kTb": dram_in(nc, "kTb", [S // 128, 128, 8, 128], BF16),
         "v": dram_in(nc, "v", [S, 1024], BF16), "kiT": dram_in(nc, "kiT", [64, S], BF16),
         "fixedsel": dram_in(nc, "fixedsel", [128, 1024]), "headsel": dram_in(nc, "headsel", [8, 128]),
         "ident": dram_in(nc, "ident", [128, 128]), "cmask": dram_in(nc, "cmask", [128, 512]),
         "dbase": dram_in(nc, "dbase", [128, A_NEAR * 128]), "coff": dram_in(nc, "coff", [128, 1]),
         "relt": dram_in(nc, "relt", [128, 256])}
    O = {"oT": dram_out(nc, "oT", [8, 128, T], BF16)}
    if dbg is not None:
        O.update({"dbg_slot": dbg, "dbg_sc": dram_out(nc, "dbg_sc", [128, S]), "dbg_mt": dram_out(nc, "dbg_mt", [128, S]),
                  "dbg_bt": dram_out(nc, "dbg_bt", [128, A_NEAR, 128]), "dbg_sm": dram_out(nc, "dbg_sm", [128, 16]),
                  "dbg_lo": dram_out(nc, "dbg_lo", [128, 256])})
    c = Ctx(nc, 512)
    a_attention(c, I, O, S, T)
    c.p.wait_bufs("sp", ["o_oT"])
    c.p.emit()
    return nc, c


def a_consts(cc):
    p = np.arange(128)
    fs = np.zeros((128, 8, 128), np.float32)
    for j in range(8):
        fs[p, j, 16 * j + (p % 16)] = 1.0
    hs = (p[None, :] // 16 == np.arange(8)[:, None]).astype(np.float32)
    q = np.arange(128)[:, None]
    k = np.arange(128)[None, :]
    cm = np.zeros((128, 4, 128), np.float32)
    for b in range(4):
        if b == cc:
            cm[:, b, :] = np.where(k <= q, 0.0, -1e30)
        elif b > cc:
            cm[:, b, :] = -1e30
    kk = np.arange(128)[:, None, None]
    ee = np.arange(A_NEAR)[None, :, None]
    qq = np.arange(128)[None, None, :]
    dbase = (128 * ee + qq - kk).astype(np.float32).reshape(128, A_NEAR * 128)
    return {"fixedsel": fs.reshape(128, 1024), "headsel": hs, "ident": np.eye(128, dtype=np.float32),
            "cmask": cm.reshape(128, 512), "dbase": dbase, "coff": np.full((128, 1), 128.0 * (cc - 3), np.float32)}


def interleave_tokens(xb, cc):
    S = xb.shape[0]
    return np.ascontiguousarray(xb.reshape(S // 128 // CPB, CPB, 128, *xb.shape[1:])[:, cc].reshape(S // CPB, *xb.shape[1:]))


def deinterleave_blocks(per_core):
    a = np.stack(per_core, axis=1)
    return np.ascontiguousarray(a.reshape(a.shape[0] * a.shape[1], *a.shape[2:]))


def linear_in2(c, w_ap, npairs, epi):
    p = c.p
    for pr in range(npairs):
        wb = c.nxt("wgu")
        wgu = c.wgu[wb]
        p.dma("pool", wgu[:, :, 0:256], w_ap[pr], f"wgu{wb}", w=[f"wgu{wb}"])
        for g in range(2):
            ch = pr * 2 + g
            for s in range(c.NS):
                sl = slice(s * 512, (s + 1) * 512)
                b = c.nxt("pm", 4)
                ps = c.pm[b]
                for kc in range(KC):
                    p.op("pe", lambda e, kc=kc, ps=ps, wgu=wgu, sl=sl, g=g: e.matmul(ps, wgu[:, kc, g * 128:(g + 1) * 128], c.xn[:, kc, sl], start=(kc == 0), stop=(kc == KC - 1)),
                         r=[f"xn{kc}", f"wgu{wb}"], w=[f"pm{b}"])
                epi(ch, s, sl, ps, f"pm{b}")


def b_map(c):
    o = c.o_misc
    m = {}
    m["vtm"] = c.view(o, [128, 4, 3072], BF16); o += 24576
    m["vn"] = c.view(o, [128, 3072], BF16); o += 6144
    m["Cc"] = c.view(o, [128, 24, 128], F32); o += 12288
    m["WtT"] = c.view(o, [128, 8, 128], BF16); o += 2048
    m["brow"] = c.view(o, [1, 3072], BF16); o += 6144
    m["lngB"] = c.view(o, [128, 3072], BF16); o += 6144
    m["bucol"] = c.view(o, [128, 24], F32); o += 96
    m["lnbc"] = c.view(o, [128, 24], F32); o += 96
    m["st"] = c.view(o, [128, 8], F32); o += 32
    assert o <= RAW_BYTES, o
    m["bspt"] = c.view(c.o_misc, [128, 8, 128], F32)
    m["tril"] = c.view(c.o_misc + 4096, [128, 128], F32)
    return m


def b_setup(c, W):
    p = c.p
    m = b_map(c)
    p.dma("pool", m["WtT"], W["wspT"].rearrange("p (g t) -> p g t", g=8), "b_wsp", w=["WtT"])
    p.dma("sp", m["tril"], W["trilT"], "b_tril", w=["tril"])
    p.dma("sp", m["bspt"], W["bsp"].rearrange("p (g t) -> p g t", g=8), "b_bsp", w=["bspt"])
    p.dma("pool", m["brow"], W["bv"], "b_bv", w=["brow"])
    p.dma("pool", m["lngB"], W["lng"], "b_lng", w=["lngB"])
    p.dma("sp", m["bucol"], W["bu"], "b_bu", w=["bucol"])
    p.dma("sp", m["lnbc"], W["lnb"], "b_lnb", w=["lnbc"])
    for g in range(8):
        p.op("dve", lambda e, g=g: e.tensor_tensor(out=m["WtT"][:, g, :], in0=m["WtT"][:, g, :], in1=m["tril"], op=ALU.mult), r=["WtT", "tril"], w=["WtT"])
    rs = c.PS[:, 0:1024]
    for g in range(8):
        p.op("pe", lambda e, g=g: e.matmul(rs[:, g * 128:(g + 1) * 128], c.ones, m["WtT"][:, g, :], start=True, stop=True), r=["ones", "WtT"], w=["pm0", "pm1"])
    for j in range(24):
        g = j // 3
        p.op("dve", lambda e, j=j, g=g: e.scalar_tensor_tensor(out=m["Cc"][:, j, :], in0=rs[:, g * 128:(g + 1) * 128], scalar=m["lnbc"][:, j:j + 1], in1=m["bspt"][:, g, :],
                                                            op0=ALU.mult, op1=ALU.add), r=["pm0", "pm1", "lnbc", "bspt"], w=["Cc"])
    p.barrier()
    return m


def mixer_b(c, m, W):
    p = c.p

    def epi_u(ch, s, sl, ps, key):
        p.op("act", lambda e: e.activation(out=c.act[:, ch, sl], in_=ps, func=AF.Gelu_apprx_tanh, bias=m["bucol"][:, ch:ch + 1]), r=[key, "bucol"], w=[f"act{ch}"])

    linear_in2(c, W["wu"], 12, epi_u)
    vtm, vn, st = m["vtm"], m["vn"], m["st"]
    for half in range(c.TT // 512):
        for cg in range(6):
            wb = c.nxt("wgu")
            wgu = c.wgu[wb]
            p.dma("pool", wgu, W["wv"][cg], f"wgu{wb}", w=[f"wgu{wb}"])
            for tc in range(4):
                tok = slice(half * 512 + tc * 128, half * 512 + (tc + 1) * 128)
                b = c.nxt("pm", 4)
                ps = c.pm[b]
                p.op("pe", lambda e, ps=ps, cg=cg: e.matmul(ps, c.ones[0:1, :], m["brow"][0:1, cg * 512:(cg + 1) * 512], start=True, stop=False),
                     r=["ones", "brow"], w=[f"pm{b}"])
                for kc in range(KC):
                    p.op("pe", lambda e, kc=kc, ps=ps, wgu=wgu, tok=tok: e.matmul(ps, c.xn[:, kc, tok], wgu[:, kc, :], start=False, stop=(kc == KC - 1)),
                         r=[f"xn{kc}", f"wgu{wb}"], w=[f"pm{b}"])
                p.op("act", lambda e, ps=ps, tc=tc, cg=cg: e.activation(out=vtm[:, tc, cg * 512:(cg + 1) * 512], in_=ps, func=AF.Gelu_apprx_tanh), r=[f"pm{b}"], w=[f"vtm{tc}"])
        for tc in range(4):
            tok0 = half * 512 + tc * 128
            p.op("dve", lambda e, tc=tc: e.tensor_reduce(out=st[:, 0:1], in_=vtm[:, tc, :], axis=AX.X, op=ALU.add), r=[f"vtm{tc}"], w=["bst0"])
            p.op("dve", lambda e: e.memset(st[:, 1:2], 0.0), w=["bst1"])
            p.op("act", lambda e, tc=tc: e.activation(out=vn, in_=vtm[:, tc, :], func=AF.Square, accum_out=st[:, 1:2]), r=[f"vtm{tc}", "bst1"], w=["vn", "bst1"])
            p.op("dve", lambda e: e.tensor_scalar(out=st[:, 2:3], in0=st[:, 0:1], scalar1=1.0 / 3072, scalar2=None, op0=ALU.mult), r=["bst0"], w=["bst2"])
            p.op("dve", lambda e: e.tensor_tensor(out=st[:, 3:4], in0=st[:, 2:3], in1=st[:, 2:3], op=ALU.mult), r=["bst2"], w=["bst3"])
            p.op("dve", lambda e: e.scalar_tensor_tensor(out=st[:, 4:5], in0=st[:, 1:2], scalar=1.0 / 3072, in1=st[:, 3:4], op0=ALU.mult, op1=ALU.subtract), r=["bst1", "bst3"], w=["bst4"])
            p.op("act", lambda e: e.activation(out=st[:, 5:6], in_=st[:, 4:5], func=AF.Sqrt, bias=EPS, scale=1.0), r=["bst4"], w=["bst5"])
            p.op("dve", lambda e: e.reciprocal(out=st[:, 6:7], in_=st[:, 5:6]), r=["bst5"], w=["bst6"])
            p.op("dve", lambda e, tc=tc: e.tensor_scalar(out=vn, in0=vtm[:, tc, :], scalar1=st[:, 2:3], scalar2=st[:, 6:7], op0=ALU.subtract, op1=ALU.mult),
                 r=[f"vtm{tc}", "bst2", "bst6"], w=["vn"])
            p.op("dve", lambda e: e.tensor_tensor(out=vn, in0=vn, in1=m["lngB"], op=ALU.mult), r=["vn", "lngB"], w=["vn"])
            for j4 in range(6):
                b = c.nxt("pm", 4)
                ps = c.pm[b]
                for jj in range(4):
                    j = j4 * 4 + jj
                    p.op("pe", lambda e, ps=ps, jj=jj, j=j: e.matmul(ps[:, jj * 128:(jj + 1) * 128], vn[:, j * 128:(j + 1) * 128], m["WtT"][:, j // 3, :], start=True, stop=True),
                         r=["vn", "WtT"], w=[f"pm{b}"])
                tb = c.nxt("tmp")
                tmp = c.tmp[tb]
                p.op("dve", lambda e, ps=ps, tmp=tmp, j4=j4: e.tensor_tensor(out=tmp.rearrange("p (a b) -> p a b", a=4), in0=ps.rearrange("p (a b) -> p a b", a=4),
                                                                        in1=m["Cc"][:, j4 * 4:(j4 + 1) * 4, :], op=ALU.add), r=[f"pm{b}", "Cc"], w=[f"tmp{tb}"])
                p.op("dve", lambda e, tmp=tmp, j4=j4, tok0=tok0: e.tensor_tensor(out=c.act[:, j4 * 4:(j4 + 1) * 4, tok0:tok0 + 128], in0=tmp.rearrange("p (a b) -> p a b", a=4),
                                                                            in1=c.act[:, j4 * 4:(j4 + 1) * 4, tok0:tok0 + 128], op=ALU.mult),
                     r=[f"tmp{tb}"] + ACTK[j4 * 4:(j4 + 1) * 4], w=ACTK[j4 * 4:(j4 + 1) * 4])
    linear_out(c, W["wout"], 24, lambda k, sl: c.act[:, k, sl], lambda k: f"act{k}", res_add_epi(c))


def c_glu(c, W, cv, y_out, t0):
    p = c.p
    yv = c.view(c.o_act, [128, KC, c.TT], F32)

    def epi(j, s, sl, pa, pg, ka, kg):
        tb = c.nxt("tmp")
        tmp = c.tmp[tb]
        p.op("act", lambda e: e.activation(out=tmp, in_=pg, func=AF.Sigmoid, bias=cv[:, 1, j:j + 1]), r=[kg, "cvec"], w=[f"tmp{tb}"])
        p.op("dve", lambda e: e.scalar_tensor_tensor(out=yv[:, j, sl], in0=pa, scalar=cv[:, 0, j:j + 1], in1=tmp, op0=ALU.add, op1=ALU.mult),
             r=[ka, f"tmp{tb}", "cvec"], w=ACTK)

    glu_in(c, W["pw1"], 8, epi)
    p.dma("sp", y_out[:, :, t0:t0 + c.TT].rearrange("k p t -> p k t"), yv, "st_y", r=ACTK, w=["o_y"])


def c_conv(c, W, cv, cw, ypad_ap, t0):
    p = c.p
    TT = c.TT
    yp = c.view(c.o_act, [128, KC, 30 + TT], F32)
    acc = c.view(c.o_misc, [128, KC, TT], F32)
    scr = c.view(c.o_misc + KC * TT * 4, [128, 512], F32)
    p.dma("sp", yp, ypad_ap[:, :, t0:t0 + 30 + TT].rearrange("k p t -> p k t"), "l_yp", w=ACTK)
    for kc in range(KC):
        eng = "dve"
        p.op(eng, lambda e, kc=kc: e.tensor_scalar(out=acc[:, kc, :], in0=yp[:, kc, 0:TT], scalar1=cw[:, kc, 0:1], scalar2=cv[:, 2, kc:kc + 1], op0=ALU.mult, op1=ALU.add),
             r=ACTK + ["cvec"], w=[f"acc{kc}"])
        for j in range(1, 31):
            p.op(eng, lambda e, kc=kc, j=j: e.scalar_tensor_tensor(out=acc[:, kc, :], in0=yp[:, kc, j:j + TT], scalar=cw[:, kc, j:j + 1], in1=acc[:, kc, :], op0=ALU.mult, op1=ALU.add),
                 r=["cvec", f"acc{kc}"], w=[f"acc{kc}"])
    ACC = [f"acc{k}" for k in range(KC)]
    p.op("act", lambda e: e.activation(out=c.sq, in_=acc, func=AF.Copy), r=ACC, w=["sq"])
    p.op("act", lambda e: e.activation(out=c.xn, in_=acc, func=AF.Square), r=ACC, w=XN)
    mean = [c.tmp[0], c.tmp[1]]
    rstd = [c.rstd[0], c.rstd[1]]
    for s in range(c.NS):
        sl = slice(s * 512, (s + 1) * 512)
        for kc in range(KC):
            p.op("pe", lambda e, kc=kc, sl=sl: e.matmul(c.pst[0], c.ones, c.sq[:, kc, sl], start=(kc == 0), stop=(kc == KC - 1)), r=["sq", "ones"], w=["pst0"])
        for kc in range(KC):
            p.op("pe", lambda e, kc=kc, sl=sl: e.matmul(c.pst[1], c.ones, c.xn[:, kc, sl], start=(kc == 0), stop=(kc == KC - 1)), r=[f"xn{kc}", "ones"], w=["pst1"])
        p.op("dve", lambda e, s=s: e.tensor_scalar(out=mean[s], in0=c.pst[0], scalar1=1.0 / D, scalar2=None, op0=ALU.mult), r=["pst0"], w=[f"tmp{s}"])
        p.op("dve", lambda e, s=s: e.tensor_tensor(out=scr, in0=mean[s], in1=mean[s], op=ALU.mult), r=[f"tmp{s}"], w=["scr"])
        p.op("dve", lambda e: e.scalar_tensor_tensor(out=scr, in0=c.pst[1], scalar=1.0 / D, in1=scr, op0=ALU.mult, op1=ALU.subtract), r=["pst1", "scr"], w=["scr"])
        p.op("act", lambda e: e.activation(out=scr, in_=scr, func=AF.Sqrt, bias=EPS, scale=1.0), r=["scr"], w=["scr"])
        p.op("dve", lambda e, s=s: e.reciprocal(out=rstd[s], in_=scr), r=["scr"], w=[f"rstd{s}"])
    for s in range(c.NS):
        sl = slice(s * 512, (s + 1) * 512)
        for kc in range(KC):
            eng = "dve" if kc % 2 == 0 else "pool"
            p.op("dve", lambda e, kc=kc, sl=sl, s=s: e.tensor_tensor(out=acc[:, kc, sl], in0=acc[:, kc, sl], in1=mean[s], op=ALU.subtract), r=[f"acc{kc}", f"tmp{s}"], w=[f"acc{kc}"])
            p.op(eng, lambda e, kc=kc, sl=sl, s=s: e.tensor_tensor(out=acc[:, kc, sl], in0=acc[:, kc, sl], in1=rstd[s], op=ALU.mult), r=[f"acc{kc}", f"rstd{s}"], w=[f"acc{kc}"])
            p.op("act", lambda e, kc=kc, sl=sl: e.activation(out=c.xn[:, kc, sl], in_=acc[:, kc, sl], func=AF.Silu, scale=cv[:, 3, kc:kc + 1], bias=cv[:, 4, kc:kc + 1]),
                 r=[f"acc{kc}", "cvec"], w=[f"xn{kc}"])
    linear_out(c, W["pw2"], 8, lambda k, sl: c.xn[:, k, sl], lambda k: f"xn{k}", res_add_epi(c, lambda oc: cv[:, 5, oc:oc + 1]))


def dproj(c, W, O, t0):
    p = c.p
    TT = c.TT
    nb = TT // 128
    vst = c.view(c.o_misc, [128, nb, 1536], BF16)
    ACC = [f"acc{k}" for k in range(KC)]

    def epi(ch, s, sl, ps, key):
        evac(c, c.act[:, ch, sl], ps, [key], [f"act{ch}"])

    linear_in2(c, W["wqk"], 12, epi)
    for cg in range(3):
        wb = c.nxt("wgu")
        wgu = c.wgu[wb]
        p.dma("pool", wgu, W["wv"][cg], f"wgu{wb}", w=[f"wgu{wb}"])
        for tb in range(nb):
            b = c.nxt("pm", 4)
            ps = c.pm[b]
            for kc in range(KC):
                p.op("pe", lambda e, kc=kc, ps=ps, wgu=wgu, tb=tb: e.matmul(ps, c.xn[:, kc, tb * 128:(tb + 1) * 128], wgu[:, kc, :], start=(kc == 0), stop=(kc == KC - 1)),
                     r=[f"xn{kc}", f"wgu{wb}"], w=[f"pm{b}"])
            evac(c, vst[:, tb, cg * 512:(cg + 1) * 512], ps, [f"pm{b}"], ACC)
    p.dma("sp", O["qkT"][:, :, t0:t0 + TT].rearrange("c p t -> p c t"), c.act, "st_qk", r=ACTK, w=["o_qk"])
    p.dma("sp", O["vD"][t0:t0 + TT, :].rearrange("(b p) n -> p b n", p=128), vst, "st_vd", r=ACC, w=["o_vd"])


D_PAIRS = ((128, 1), (512, 4), (2048, 16))
D_SCALE = 64 ** -0.5
D_HALO = 2048


def d_attention(c, I, O, T):
    p = c.p
    SP = 2048
    o = 0
    qs = c.view(o, [128, 2, SP], BF16); o += 2 * SP * 2
    ks = c.view(o, [128, 2, 2 * SP], BF16); o += 4 * SP * 2
    vb = [[c.view(o + (i * 2 + kb) * 512, [128, 256], BF16) for kb in range(2)] for i in range(2)]; o += 2048
    num = c.view(o, [64, 4, SP], F32); o += 4 * SP * 4
    den = c.view(o, [64, 4, SP], F32); o += 4 * SP * 4
    btD = c.view(o, [128, 6, 8, 128], BF16); o += 6 * 8 * 256
    btD0 = c.view(o, [128, 3, 8, 128], BF16); o += 3 * 8 * 256
    pT = [c.view(o + i * 256, [128, 128], BF16) for i in range(4)]; o += 1024
    identb = c.view(o, [128, 128], BF16); o += 256
    onesb = c.view(o, [128, 64], BF16); o += 128
    relt = c.view(o, [128, 256], F32); o += 1024
    delta = c.view(o, [128, 256], F32); o += 1024
    DT = c.view(o, [128, 128], F32); o += 512
    Mt = c.view(o, [128, 128], F32); o += 512
    Fg = c.view(o, [128, 8, 128], F32); o += 4096
    dqk = c.view(o, [128, 128], F32); o += 512
    wm = c.view(o, [128, 2, 128], F32); o += 1024
    negbig = c.view(o, [128, 1], F32); o += 4
    yD = c.view(o, [64, 4, SP], BF16); o += 4 * SP * 2
    rden = [c.view(o + i * 2048, [64, 512], F32) for i in range(2)]; o += 4096
    assert o <= RAW_BYTES, o
    STr = [c.PS[:, i * 128:(i + 1) * 128] for i in range(4)]
    NUMr = [c.PS[0:64, 512 + i * 128: 512 + (i + 1) * 128] for i in range(4)]
    DENr = [c.PS[0:64, 1024 + i * 128: 1024 + (i + 1) * 128] for i in range(4)]
    p.dma("pool", identb, I["ident"], "c_id", w=["identb"])
    p.dma("sp", dqk, I["dqk"], "c_dqk", w=["dqk"])
    p.dma("sp", wm, I["wm"].rearrange("p (a b) -> p a b", a=2), "c_wm", w=["wm"])
    p.dma("sp", negbig, I["negbig"], "c_nb", w=["negbig"])
    p.op("dve", lambda e: e.memset(onesb, 1.0), w=["onesb"])
    load_relt(c, relt, delta, I["relt"])
    for g, (window, dil) in enumerate(D_PAIRS):
        for kb in range(2):
            p.op("dve", lambda e, kb=kb, dil=dil: e.tensor_scalar(out=DT, in0=dqk, scalar1=128.0 * (1 - kb), scalar2=float(dil), op0=ALU.add, op1=ALU.mult),
                 r=["dqk", "Mt"] + [f"F{h}" for h in range(8)], w=["DT"])
            gen_bias(c, DT, Mt, Fg, relt, delta, 128)
            for h in range(8):
                p.op("dve", lambda e, g=g, kb=kb, h=h: e.scalar_tensor_tensor(out=btD[:, g * 2 + kb, h, :], in0=Fg[:, h, :], scalar=1.0 / D_SCALE, in1=wm[:, kb, :], op0=ALU.mult, op1=ALU.add),
                     r=[f"F{h}", "wm"], w=["btD"])
        p.op("dve", lambda e, g=g: e.tensor_scalar(out=btD0[:, g, :, :], in0=btD[:, g * 2, :, :], scalar1=negbig, scalar2=None, op0=ALU.add), r=["btD", "negbig"], w=["btD0"])
    HB = D_HALO
    for sp in range(T // SP):
        for hh in range(2):
            for g, (window, dil) in enumerate(D_PAIRS):
                gspan = 128 * dil
                base = HB + sp * SP
                p.dma("sp", qs, I["qkT"][g * 4 + hh * 2: g * 4 + hh * 2 + 2, :, base:base + SP].rearrange("c p t -> p c t"), "l_qs", w=["qs"])
                p.dma("sp", ks, I["qkT"][12 + g * 4 + hh * 2: 12 + g * 4 + hh * 2 + 2, :, base - SP:base + SP].rearrange("c p t -> p c t"), "l_ks", w=["ks"])
                for u in range(SP // gspan):
                    for r_ in range(dil):
                        q0 = u * gspan + r_
                        qtok = slice(q0, q0 + 127 * dil + 1, dil)
                        vi = c.nxt("vb")
                        for kb in range(2):
                            k0 = SP + q0 - (1 - kb) * gspan
                            row0 = base - SP + k0
                            src = I["vD"][row0: row0 + 127 * dil + 1: dil, g * 512 + hh * 256: g * 512 + hh * 256 + 256]
                            p.dma("sp", vb[vi][kb], src, f"l_vb{vi}{kb}", w=[f"vb{vi}{kb}"])
                        first = (sp == 0 and u == 0)
                        for hl in range(4):
                            h = hh * 4 + hl
                            hp = hl % 2
                            ch = hl // 2
                            ni = c.nxt("nd", 4)
                            for kb in range(2):
                                k0 = SP + q0 - (1 - kb) * gspan
                                ktok = slice(k0, k0 + 127 * dil + 1, dil)
                                si = c.nxt("st", 4)
                                st = STr[si]
                                bt = btD0[:, g, h, :] if (first and kb == 0) else btD[:, g * 2 + kb, h, :]
                                p.op("pe", lambda e, st=st, hp=hp, ch=ch, ktok=ktok, qtok=qtok: e.matmul(st, ks[hp * 64:(hp + 1) * 64, ch, ktok], qs[hp * 64:(hp + 1) * 64, ch, qtok], start=True, stop=False),
                                     r=["ks", "qs"], w=[f"st{si}"])
                                p.op("pe", lambda e, st=st, bt=bt: e.matmul(st, identb, bt, start=False, stop=True), r=["identb", "btD", "btD0"], w=[f"st{si}"])
                                pi = c.nxt("pT", 4)
                                p.op("act", lambda e, st=st, pi=pi: e.activation(out=pT[pi], in_=st, func=AF.Exp, scale=D_SCALE), r=[f"st{si}"], w=[f"pT{pi}"])
                                p.op("pe", lambda e, vi=vi, kb=kb, hl=hl, pi=pi, ni=ni: e.matmul(NUMr[ni], vb[vi][kb][:, hl * 64:(hl + 1) * 64], pT[pi], start=(kb == 0), stop=(kb == 1)),
                                     r=[f"vb{vi}{kb}", f"pT{pi}"], w=[f"num{ni}"])
                                p.op("pe", lambda e, pi=pi, ni=ni, kb=kb: e.matmul(DENr[ni], onesb, pT[pi], start=(kb == 0), stop=(kb == 1)),
                                     r=["onesb", f"pT{pi}"], w=[f"den{ni}"])
                            if g == 0:
                                p.op("dve", lambda e, hl=hl, qtok=qtok, ni=ni: e.tensor_copy(out=num[:, hl, qtok], in_=NUMr[ni]), r=[f"num{ni}"], w=[f"nacc{hl}"])
                                p.op("dve", lambda e, hl=hl, qtok=qtok, ni=ni: e.tensor_copy(out=den[:, hl, qtok], in_=DENr[ni]), r=[f"den{ni}"], w=[f"dacc{hl}"])
                            else:
                                p.op("dve", lambda e, hl=hl, qtok=qtok, ni=ni: e.tensor_tensor(out=num[:, hl, qtok], in0=NUMr[ni], in1=num[:, hl, qtok], op=ALU.add),
                                     r=[f"num{ni}", f"nacc{hl}"], w=[f"nacc{hl}"])
                                p.op("dve", lambda e, hl=hl, qtok=qtok, ni=ni: e.tensor_tensor(out=den[:, hl, qtok], in0=DENr[ni], in1=den[:, hl, qtok], op=ALU.add),
                                     r=[f"den{ni}", f"dacc{hl}"], w=[f"dacc{hl}"])
            for hl in range(4):
                for s in range(SP // 512):
                    sl = slice(s * 512, (s + 1) * 512)
                    ri = c.nxt("rden")
                    p.op("dve", lambda e, hl=hl, sl=sl, ri=ri: e.reciprocal(out=rden[ri], in_=den[:, hl, sl]), r=[f"dacc{hl}"], w=[f"rden{ri}"])
                    p.op("dve", lambda e, hl=hl, sl=sl, ri=ri: e.tensor_tensor(out=yD[:, hl, sl], in0=num[:, hl, sl], in1=rden[ri], op=ALU.mult), r=[f"nacc{hl}", f"rden{ri}"], w=["yD"])
            p.dma("sp", O["yD"][hh * 4:(hh + 1) * 4, :, sp * SP:(sp + 1) * SP].rearrange("h p t -> p h t"), yD, "st_yd", r=["yD"], w=["o_yD"])


def d_out(c, W, yD_ap, t0):
    p = c.p
    yv = c.view(KC * c.TT * 4, [64, 8, c.TT], BF16)
    p.dma("sp", yv, yD_ap[:, :, t0:t0 + c.TT].rearrange("h p t -> p h t"), "l_yd", r=["dram_yD"], w=XN)
    for oc in range(KC):
        wb = c.nxt("wo")
        wo = c.wo[wb]
        p.dma("pool", wo[0:64, 0:8, :], W["dwout"][oc], f"wo{wb}", w=[f"wo{wb}"])
        for s in range(c.NS):
            sl = slice(s * 512, (s + 1) * 512)
            b = c.nxt("po")
            po = c.po[b]
            for k in range(8):
                p.op("pe", lambda e, k=k, po=po, wo=wo, sl=sl: e.matmul(po, wo[0:64, k, :], yv[:, k, sl], start=(k == 0), stop=(k == 7)),
                     r=XN + [f"wo{wb}"], w=[f"po{b}"])
            p.op("dve", lambda e, po=po, oc=oc, sl=sl: e.tensor_tensor(out=c.ht[:, oc, sl], in0=po, in1=c.ht[:, oc, sl], op=ALU.add),
                 r=[f"po{b}", f"ht{oc}"], w=[f"ht{oc}"])


def final_norm(c, gi, out_ap, t0):
    p = c.p
    ov = c.view(c.o_act, [128, KC, c.TT], F32)
    p.op("act", lambda e: e.activation(out=c.sq, in_=c.ht, func=AF.Square), r=HT, w=["sq"])
    for s in range(c.NS):
        sl = slice(s * 512, (s + 1) * 512)
        b = c.nxt("pst")
        pst = c.pst[b]
        for kc in range(KC):
            p.op("pe", lambda e, kc=kc, pst=pst, sl=sl: e.matmul(pst, c.ones, c.sq[:, kc, sl], start=(kc == 0), stop=(kc == KC - 1)), r=["sq", "ones"], w=[f"pst{b}"])
        rb = c.nxt("rstd")
        tb = c.nxt("tmp")
        p.op("act", lambda e, pst=pst, tb=tb: e.activation(out=c.tmp[tb], in_=pst, func=AF.Sqrt, bias=EPS, scale=1.0 / D), r=[f"pst{b}"], w=[f"tmp{tb}"])
        p.op("dve", lambda e, rb=rb, tb=tb: e.reciprocal(out=c.rstd[rb], in_=c.tmp[tb]), r=[f"tmp{tb}"], w=[f"rstd{rb}"])
        for kc in range(KC):
            p.op("dve", lambda e, kc=kc, rb=rb, sl=sl: e.scalar_tensor_tensor(out=ov[:, kc, sl], in0=c.ht[:, kc, sl], scalar=c.gcols[:, gi, kc:kc + 1], in1=c.rstd[rb], op0=ALU.mult, op1=ALU.mult),
                 r=[f"ht{kc}", f"rstd{rb}", "gcols"], w=ACTK)
    p.dma("sp", out_ap[:, :, t0:t0 + c.TT].rearrange("k p t -> p k t"), ov, "st_out", r=ACTK, w=["o_out"])


def ffn_inputs(nc, tag):
    return (dram_in(nc, f"win_{tag}", [NFF, 128, KC, 256]), dram_in(nc, f"wout_{tag}", [KC, 128, NFF, 128]))


def build_L2(S, T):
    nc = bass.Bass("TRN2", target_bir_lowering=False)
    TT = min(1024, T)
    I = {"qT": dram_in(nc, "qT", [8, 128, T], BF16), "qiT": dram_in(nc, "qiT", [64, T * 8], BF16),
         "wiT": dram_in(nc, "wiT", [8, T], F32), "kTb": dram_in(nc, "kTb", [S // 128, 128, 8, 128], BF16),
         "v": dram_in(nc, "v", [S, 1024], BF16), "kiT": dram_in(nc, "kiT", [64, S], BF16),
         "fixedsel": dram_in(nc, "fixedsel", [128, 1024]), "headsel": dram_in(nc, "headsel", [8, 128]),
         "ident": dram_in(nc, "ident", [128, 128]), "cmask": dram_in(nc, "cmask", [128, 512]),
         "dbase": dram_in(nc, "dbase", [128, A_NEAR * 128]), "coff": dram_in(nc, "coff", [128, 1]),
         "relt": dram_in(nc, "relt", [128, 256])}
    oT = nc.dram_tensor("oT_scr", [8, 128, T], BF16, kind="Internal").ap()
    hin = dram_in(nc, "hT_in", [KC, 128, T])
    gv = dram_in(nc, "gv", [128, 6, KC])
    awout = dram_in(nc, "awout", [KC, 128, 8, 128])
    F01 = ffn_inputs(nc, "01"); F10 = ffn_inputs(nc, "10"); F11 = ffn_inputs(nc, "11"); F20 = ffn_inputs(nc, "20")
    WB = {"wu": dram_in(nc, "b_wu", [12, 128, KC, 256]), "wv": dram_in(nc, "b_wv", [6, 128, KC, 512]), "wout": dram_in(nc, "b_wout", [KC, 128, 24, 128]),
          "wspT": dram_in(nc, "b_wspT", [128, 1024]), "trilT": dram_in(nc, "b_trilT", [128, 128]), "bsp": dram_in(nc, "b_bsp", [128, 1024]),
          "bv": dram_in(nc, "b_bv", [1, 3072]), "lng": dram_in(nc, "b_lng", [128, 3072]), "bu": dram_in(nc, "b_bu", [128, 24]), "lnb": dram_in(nc, "b_lnb", [128, 24])}
    WC = {"pw1": dram_in(nc, "c_pw1", [8, 128, KC, 256])}
    cvec = dram_in(nc, "c_vec", [128, 8, 8])
    hout = dram_out(nc, "hT", [KC, 128, T])
    yout = dram_out(nc, "yT", [KC, 128, T])
    c = Ctx(nc, TT)
    a_attention(c, I, {"oT": oT}, S, T)
    c.p.barrier()
    c.init_consts()
    load_gcols(c, gv, 6)
    cv = c.view(c.o_misc + 60000 - 512, [128, 8, 8], F32) if False else None
    m = b_setup(c, WB)
    cvv = c.view(RAW_BYTES - 256, [128, 8, 8], F32)
    c.p.dma("sp", cvv, cvec, "cvec", w=["cvec"])
    for t0 in range(0, T, TT):
        load_ht(c, hin, t0)
        c.p.dma("sp", c.xn, oT[:, :, t0:t0 + TT].rearrange("h p t -> p h t"), "l_oT", r=["o_oT"], w=XN)
        linear_out(c, awout, 8, lambda k, sl: c.xn[:, k, sl], lambda k: f"xn{k}", res_add_epi(c))
        rmsnorm(c, 0); ffn(c, *F01)
        rmsnorm(c, 1); ffn(c, *F10)
        rmsnorm(c, 2); mixer_b(c, m, WB)
        rmsnorm(c, 3); ffn(c, *F11)
        rmsnorm(c, 4); ffn(c, *F20)
        store_ht(c, hout, t0, key="o_h")
        rmsnorm(c, 5)
        c_glu(c, WC, cvv, yout, t0)
    c.p.wait_bufs("sp", ["o_h", "o_y"])
    c.p.emit()
    return nc, c


def build_L3(T):
    nc = bass.Bass("TRN2", target_bir_lowering=False)
    TT = min(1024, T)
    hin = dram_in(nc, "hT_in", [KC, 128, T])
    ypad = dram_in(nc, "ypad", [KC, 128, 30 + T])
    gv = dram_in(nc, "gv", [128, 3, KC])
    cvec = dram_in(nc, "c_vec", [128, 8, 8])
    cwdw = dram_in(nc, "c_wdw", [128, 8, 31])
    WC = {"pw2": dram_in(nc, "c_pw2", [KC, 128, 8, 128])}
    F21 = ffn_inputs(nc, "21"); F30 = ffn_inputs(nc, "30")
    WD = {"wqk": dram_in(nc, "d_wqk", [12, 128, KC, 256]), "wv": dram_in(nc, "d_wv", [3, 128, KC, 512])}
    hout = dram_out(nc, "hT", [KC, 128, T])
    O = {"qkT": dram_out(nc, "qkT", [24, 128, T], BF16), "vD": dram_out(nc, "vD", [T, 1536], BF16)}
    c = Ctx(nc, TT)
    c.init_consts()
    load_gcols(c, gv, 3)
    cvv = c.view(RAW_BYTES - 256, [128, 8, 8], F32)
    cw = c.view(RAW_BYTES - 256 - 992, [128, 8, 31], F32)
    c.p.dma("sp", cvv, cvec, "cvec", w=["cvec"])
    c.p.dma("sp", cw, cwdw, "cvec", w=["cvec"])
    for t0 in range(0, T, TT):
        load_ht(c, hin, t0)
        c_conv(c, WC, cvv, cw, ypad, t0)
        rmsnorm(c, 0); ffn(c, *F21)
        rmsnorm(c, 1); ffn(c, *F30)
        store_ht(c, hout, t0, key="o_h")
        rmsnorm(c, 2)
        dproj(c, WD, O, t0)
    c.p.wait_bufs("sp", ["o_h", "o_qk", "o_vd"])
    c.p.emit()
    return nc, c


def build_L4(T):
    nc = bass.Bass("TRN2", target_bir_lowering=False)
    TT = min(1024, T)
    I = {"qkT": dram_in(nc, "qkT", [24, 128, D_HALO + T], BF16), "vD": dram_in(nc, "vD", [D_HALO + T, 1536], BF16),
         "ident": dram_in(nc, "ident", [128, 128]), "dqk": dram_in(nc, "dqk", [128, 128]), "wm": dram_in(nc, "wm", [128, 256]),
         "negbig": dram_in(nc, "negbig", [128, 1]), "relt": dram_in(nc, "relt", [128, 256])}
    yD = nc.dram_tensor("yD_scr", [8, 64, T], BF16, kind="Internal").ap()
    hin = dram_in(nc, "hT_in", [KC, 128, T])
    gv = dram_in(nc, "gv", [128, 2, KC])
    WD = {"dwout": dram_in(nc, "d_wout", [KC, 64, 8, 128])}
    F31 = ffn_inputs(nc, "31")
    out = dram_out(nc, "outT", [KC, 128, T])
    c = Ctx(nc, TT)
    d_attention(c, I, {"yD": yD}, T)
    c.p.barrier()
    c.init_consts()
    load_gcols(c, gv, 2)
    for t0 in range(0, T, TT):
        load_ht(c, hin, t0)
        d_out(c, WD, yD, t0)
        rmsnorm(c, 0); ffn(c, *F31)
        final_norm(c, 1, out, t0)
    c.p.wait_bufs("sp", ["o_out"])
    c.p.emit()
    return nc, c


_PROGS = {}


def _prog(name, builder, *args):
    key = (name,) + args
    if key not in _PROGS:
        _PROGS[key] = builder(*args)[0]
    return _PROGS[key]


def _rep(v, n=128):
    return np.ascontiguousarray(np.broadcast_to(np.asarray(v, np.float32).reshape(1, -1), (n, v.size)))


def _ffn_w(inp, i, k, tag):
    return {f"win_{tag}": lay_pair(inp["ffn_w_in"][i, k], DFF), f"wout_{tag}": lay_out(inp["ffn_w_out"][i, k])}


def _fm_full(per_core_blocks_interleaved, S):
    a = np.stack([x.reshape(x.shape[0], -1, 128) for x in per_core_blocks_interleaved], axis=2)
    return np.ascontiguousarray(a.reshape(a.shape[0], S))


def run_model(inp, B, S, stages=None, dump=None):
    inp = {k: np.asarray(v, dtype=np.float32) for k, v in inp.items()}
    T = S // CPB
    ncores = B * CPB
    cores = list(range(ncores))
    x = inp["x"]
    ng = inp["norm_g"]
    relt = _rep(inp["rel_table"].reshape(-1))
    ident = np.eye(128, dtype=np.float32)
    aw = inp["a_w_in"][0]
    w1 = {"gv": lay_vec(ng[0, 0:2]),
          "wq": lay_cols(aw[:, 0:1024], 256), "wk": lay_cols(aw[:, 1024:2048], 256), "wv": lay_cols(aw[:, 2048:3072], 512),
          "wqi": lay_cols(aw[:, 3072:3584], 512), "wkw": lay_cols(aw[:, 3584:3656], 72)}
    f00 = _ffn_w(inp, 0, 0, "x")
    w1["win"] = f00["win_x"]; w1["wout"] = f00["wout_x"]
    ims = [dict(w1, xT=lay_xT(interleave_tokens(x[c // CPB], c % CPB))) for c in cores]
    r1 = run_bass_kernel_spmd(_prog("L1", build_L1, T), ims, core_ids=cores).results
    if dump is not None:
        dump["r1"] = r1
    w2 = {"gv": lay_vec(np.stack([ng[0, 2], ng[1, 0], ng[1, 1], ng[1, 2], ng[2, 0], ng[2, 1]])),
          "awout": lay_out(inp["a_w_out"][0]), "relt": relt}
    w2.update(_ffn_w(inp, 0, 1, "01")); w2.update(_ffn_w(inp, 1, 0, "10")); w2.update(_ffn_w(inp, 1, 1, "11")); w2.update(_ffn_w(inp, 2, 0, "20"))
    bw = inp["b_w_in"][0]
    sidx = np.arange(128)
    w2.update({"b_wu": lay_cols(bw[:, :3072], 256), "b_wv": lay_cols(bw[:, 3072:], 512), "b_wout": lay_out(inp["b_w_out"][0]),
               "b_wspT": np.ascontiguousarray(inp["b_w_sp"][0].transpose(2, 0, 1).reshape(128, 1024)),
               "b_trilT": (sidx[:, None] <= sidx[None, :]).astype(np.float32),
               "b_bsp": _rep(inp["b_b_sp"][0].reshape(-1)), "b_bv": np.ascontiguousarray(inp["b_b_in"][0][3072:][None]),
               "b_lng": _rep(inp["b_ln_g"][0]), "b_bu": np.ascontiguousarray(lay_vec(inp["b_b_in"][0][None, :3072])[:, 0, :]),
               "b_lnb": np.ascontiguousarray(lay_vec(inp["b_ln_b"][0][None])[:, 0, :]),
               "c_pw1": lay_pair(inp["c_w_pw1"][0], 1024)})
    cb = inp["c_b_pw1"][0]
    cvec = lay_vec(np.stack([cb[:1024], cb[1024:], inp["c_b_dw"][0], inp["c_ln_g"][0], inp["c_ln_b"][0], inp["c_b_pw2"][0],
                             np.zeros(1024, np.float32), np.zeros(1024, np.float32)]))
    w2["c_vec"] = cvec
    ims = []
    for b in range(B):
        cs = [b * CPB + cc for cc in range(CPB)]
        kTb_all = deinterleave_blocks([np.asarray(r1[c]["kTb"]) for c in cs])
        v_all = deinterleave_blocks([np.asarray(r1[c]["v"]).reshape(T // 128, 128, 1024) for c in cs]).reshape(S, 1024)
        kiT_all = np.ascontiguousarray(np.stack([np.asarray(r1[c]["kiT"]).reshape(64, T // 128, 128) for c in cs], axis=2).reshape(64, S))
        for cc in range(CPB):
            c = cs[cc]
            d = dict(w2, qT=r1[c]["qT"], qiT=r1[c]["qiT"], wiT=r1[c]["wiT"], kTb=kTb_all, v=v_all, kiT=kiT_all, hT_in=r1[c]["hT"])
            d.update(a_consts(cc))
            ims.append(d)
    r2 = run_bass_kernel_spmd(_prog("L2", build_L2, S, T), ims, core_ids=cores).results
    if dump is not None:
        dump["r2"] = r2
    w3 = {"gv": lay_vec(np.stack([ng[2, 2], ng[3, 0], ng[3, 1]])), "c_vec": cvec,
          "c_wdw": np.ascontiguousarray(inp["c_w_dw"][0].T.reshape(KC, 128, 31).transpose(1, 0, 2)),
          "c_pw2": lay_out(inp["c_w_pw2"][0]),
          "d_wqk": lay_cols(inp["d_w_in"][0][:, :3072], 256), "d_wv": lay_cols(inp["d_w_in"][0][:, 3072:], 512)}
    w3.update(_ffn_w(inp, 2, 1, "21")); w3.update(_ffn_w(inp, 3, 0, "30"))
    ims = []
    for b in range(B):
        cs = [b * CPB + cc for cc in range(CPB)]
        hfull = _fm_full([np.asarray(r2[c]["hT"]).reshape(D, T) for c in cs], S)
        yfull = _fm_full([np.asarray(r2[c]["yT"]).reshape(D, T) for c in cs], S)
        ypadfull = np.concatenate([np.zeros((D, 30), np.float32), yfull], axis=1)
        for cc in range(CPB):
            ims.append(dict(w3, hT_in=np.ascontiguousarray(hfull[:, cc * T:(cc + 1) * T]).reshape(KC, 128, T),
                            ypad=np.ascontiguousarray(ypadfull[:, cc * T: cc * T + 30 + T]).reshape(KC, 128, 30 + T)))
    r3 = run_bass_kernel_spmd(_prog("L3", build_L3, T), ims, core_ids=cores).results
    if dump is not None:
        dump["r3"] = r3
    q_ = np.arange(128)[None, :]
    k_ = np.arange(128)[:, None]
    wm = np.concatenate([np.where(k_ >= q_, 0.0, -30000.0), np.where(k_ <= q_, 0.0, -30000.0)], axis=1).astype(np.float32)
    w4 = {"gv": lay_vec(np.stack([ng[3, 2], inp["final_g"]])), "ident": ident, "dqk": (q_ - k_).astype(np.float32), "wm": wm, "relt": relt,
          "d_wout": np.ascontiguousarray(inp["d_w_out"][0].reshape(8, 64, KC, 128).transpose(2, 1, 0, 3))}
    w4.update(_ffn_w(inp, 3, 1, "31"))
    ims = []
    for b in range(B):
        cs = [b * CPB + cc for cc in range(CPB)]
        qk = np.concatenate([np.asarray(r3[c]["qkT"]) for c in cs], axis=2)
        vd = np.concatenate([np.asarray(r3[c]["vD"]) for c in cs], axis=0)
        qk = np.concatenate([np.zeros((24, 128, D_HALO), qk.dtype), qk], axis=2)
        vd = np.concatenate([np.zeros((D_HALO, 1536), vd.dtype), vd], axis=0)
        for cc in range(CPB):
            c = cs[cc]
            ims.append(dict(w4, qkT=np.ascontiguousarray(qk[:, :, cc * T: cc * T + D_HALO + T]), vD=np.ascontiguousarray(vd[cc * T: cc * T + D_HALO + T]),
                            hT_in=r3[c]["hT"], negbig=np.full((128, 1), -30000.0 if cc == 0 else 0.0, np.float32)))
    r4 = run_bass_kernel_spmd(_prog("L4", build_L4, T), ims, core_ids=cores).results
    out = np.empty((B, S, D), np.float32)
    for c in cores:
        out[c // CPB, (c % CPB) * T:((c % CPB) + 1) * T] = unlay_xT(np.asarray(r4[c]["outT"]))
    return out


def kernel(**inputs):
    return run_model(inputs, 2, 16384)
```
